# Optimizing a Trainium2 kernel written in Bass

```python
import math
import jax, jax.numpy as jnp
from jax import lax
import numpy as np

D_MODEL = 1024
BATCH = 8
SEQ = 4096
DEPTH = 2

N_MIXERS = 2
CHUNK = 128
D_FF = 2816
A_WIDTH = 2 * D_MODEL
A_GROUPS = 8
A_GROUP_DIM = A_WIDTH // A_GROUPS
B_HEADS = 4
B_QK_DIM = D_MODEL // B_HEADS
B_V_DIM = 2 * B_QK_DIM
B_V_WIDTH = B_HEADS * B_V_DIM
ROPE_BASE = 10000.0
ALPHA = float((2 * DEPTH) ** 0.25)
BETA = float((8 * DEPTH) ** -0.25)
N_A = (DEPTH + 1) // 2
N_B = DEPTH // 2
LN_EPS = 1e-5
GN_EPS = 1e-6

kernel_name = "deepnorm_macaron_gmlp_retention_hybrid"


def layer_norm(x, g, b):
    xf = x.astype(jnp.float32)
    mu = jnp.mean(xf, axis=-1, keepdims=True)
    var = jnp.mean(jnp.square(xf - mu), axis=-1, keepdims=True)
    y = (xf - mu) * lax.rsqrt(var + LN_EPS)
    return (y * g.astype(jnp.float32) + b.astype(jnp.float32)).astype(x.dtype)


def swiglu(x, w_gate, w_up, w_down):
    return (jax.nn.silu(x @ w_gate) * (x @ w_up)) @ w_down


def chunked_spatial_gating(x, w_in, ln_g, ln_b, w_s, b_s, w_out):
    B, S, _ = x.shape
    nc = S // CHUNK
    z = jax.nn.gelu(x @ w_in, approximate=False)
    u, v = jnp.split(z, 2, axis=-1)
    v = layer_norm(v, ln_g, ln_b)
    v = v.reshape(B, nc, CHUNK, A_GROUPS, A_GROUP_DIM)
    causal = jnp.tril(jnp.ones((CHUNK, CHUNK), dtype=bool))
    w = jnp.where(causal[None], w_s, jnp.zeros((), w_s.dtype)).astype(v.dtype)
    mixed = jnp.einsum('gts,bcsgd->bctgd', w, v)
    mixed = mixed + b_s.T.astype(v.dtype)[None, None, :, :, None]
    gated = u * mixed.reshape(B, S, A_WIDTH)
    return gated @ w_out


def rotary(x, pos):
    half = x.shape[-1] // 2
    inv = ROPE_BASE ** (-jnp.arange(half, dtype=jnp.float32) / half)
    ang = pos.astype(jnp.float32)[:, None] * inv[None, :]
    cos = jnp.cos(ang)[None, :, None, :].astype(x.dtype)
    sin = jnp.sin(ang)[None, :, None, :].astype(x.dtype)
    x1, x2 = x[..., :half], x[..., half:]
    return jnp.concatenate([x1 * cos - x2 * sin, x1 * sin + x2 * cos], axis=-1)


def retention(x, w_in, w_out):
    B, S, _ = x.shape
    nc = S // CHUNK
    proj = x @ w_in
    q, k, v, g = jnp.split(proj, [D_MODEL, 2 * D_MODEL, 2 * D_MODEL + B_V_WIDTH], axis=-1)
    pos = jnp.arange(S)
    q = rotary(q.reshape(B, S, B_HEADS, B_QK_DIM), pos).astype(jnp.float32)
    k = (rotary(k.reshape(B, S, B_HEADS, B_QK_DIM), pos).astype(jnp.float32)
         * (B_QK_DIM ** -0.5))
    v = v.reshape(B, S, B_HEADS, B_V_DIM).astype(jnp.float32)

    log_gamma = jnp.log1p(-jnp.exp2(-5.0 - jnp.arange(B_HEADS, dtype=jnp.float32)))
    idx = jnp.arange(CHUNK, dtype=jnp.float32)
    rel = idx[:, None] - idx[None, :]
    intra_decay = jnp.where(rel >= 0,
                            jnp.exp(log_gamma[:, None, None] * jnp.maximum(rel, 0.0)),
                            0.0)
    query_decay = jnp.exp(log_gamma[:, None] * (idx + 1.0))[None, :, :, None]
    key_decay = jnp.exp(log_gamma[:, None] * (CHUNK - 1.0 - idx))[None, :, :, None]
    chunk_decay = jnp.exp(log_gamma * CHUNK)[None, :, None, None]

    def to_chunks(t):
        return t.reshape(B, nc, CHUNK, B_HEADS, t.shape[-1]).transpose(1, 0, 3, 2, 4)

    def step(state, inp):
        qi, ki, vi = inp
        scores = jnp.einsum('bhtd,bhsd->bhts', qi, ki) * intra_decay[None]
        inner = jnp.einsum('bhts,bhsv->bhtv', scores, vi)
        cross = jnp.einsum('bhtd,bhdv->bhtv', qi, state) * query_decay
        new_state = state * chunk_decay + jnp.einsum('bhsd,bhsv->bhdv', ki * key_decay, vi)
        return new_state, inner + cross

    state0 = jnp.zeros((B, B_HEADS, B_QK_DIM, B_V_DIM), jnp.float32)
    _, y = lax.scan(step, state0, (to_chunks(q), to_chunks(k), to_chunks(v)))
    y = y.transpose(1, 0, 3, 2, 4).reshape(B, S, B_HEADS, B_V_DIM)
    mu = jnp.mean(y, axis=-1, keepdims=True)
    var = jnp.mean(jnp.square(y - mu), axis=-1, keepdims=True)
    y = ((y - mu) * lax.rsqrt(var + GN_EPS)).reshape(B, S, B_V_WIDTH).astype(x.dtype)
    return (jax.nn.silu(g) * y) @ w_out


def setup_inputs(seed: int = 0) -> dict:
    key = jax.random.key(seed)
    ks = jax.random.split(key, 16)
    f32 = jnp.float32
    nrm = lambda k, shape, scale: jax.random.normal(k, shape, f32) * scale
    x = jax.random.normal(ks[0], (BATCH, SEQ, D_MODEL), f32)
    ln_g = 1.0 + nrm(ks[1], (DEPTH, 3, D_MODEL), 0.1)
    ln_b = nrm(ks[2], (DEPTH, 3, D_MODEL), 0.02)
    ffn_w_gate = nrm(ks[3], (DEPTH, 2, D_MODEL, D_FF), D_MODEL ** -0.5)
    ffn_w_up = nrm(ks[4], (DEPTH, 2, D_MODEL, D_FF), D_MODEL ** -0.5)
    ffn_w_down = nrm(ks[5], (DEPTH, 2, D_FF, D_MODEL), BETA * D_FF ** -0.5)
    a_w_in = nrm(ks[6], (N_A, D_MODEL, 2 * A_WIDTH), D_MODEL ** -0.5)
    a_ln_g = 1.0 + nrm(ks[7], (N_A, A_WIDTH), 0.1)
    a_ln_b = nrm(ks[8], (N_A, A_WIDTH), 0.02)
    a_w_s = nrm(ks[9], (N_A, A_GROUPS, CHUNK, CHUNK), CHUNK ** -0.5)
    a_b_s = 1.0 + nrm(ks[10], (N_A, A_GROUPS, CHUNK), 0.1)
    a_w_out = nrm(ks[11], (N_A, A_WIDTH, D_MODEL), BETA * A_WIDTH ** -0.5)
    b_w_in = nrm(ks[12], (N_B, D_MODEL, 2 * D_MODEL + 2 * B_V_WIDTH), D_MODEL ** -0.5)
    b_w_out = nrm(ks[13], (N_B, B_V_WIDTH, D_MODEL), BETA * B_V_WIDTH ** -0.5)
    return {"x": x, "ln_g": ln_g, "ln_b": ln_b,
            "ffn_w_gate": ffn_w_gate, "ffn_w_up": ffn_w_up, "ffn_w_down": ffn_w_down,
            "a_w_in": a_w_in, "a_ln_g": a_ln_g, "a_ln_b": a_ln_b,
            "a_w_s": a_w_s, "a_b_s": a_b_s, "a_w_out": a_w_out,
            "b_w_in": b_w_in, "b_w_out": b_w_out}


def reference(x, ln_g, ln_b, ffn_w_gate, ffn_w_up, ffn_w_down,
              a_w_in, a_ln_g, a_ln_b, a_w_s, a_b_s, a_w_out,
              b_w_in, b_w_out):
    for i in range(DEPTH):
        f = swiglu(x, ffn_w_gate[i, 0], ffn_w_up[i, 0], ffn_w_down[i, 0])
        x = layer_norm(ALPHA * x + 0.5 * f, ln_g[i, 0], ln_b[i, 0])
        j = i // N_MIXERS
        if i % N_MIXERS == 0:
            h = chunked_spatial_gating(x, a_w_in[j], a_ln_g[j], a_ln_b[j],
                                       a_w_s[j], a_b_s[j], a_w_out[j])
        else:
            h = retention(x, b_w_in[j], b_w_out[j])
        x = layer_norm(ALPHA * x + h, ln_g[i, 1], ln_b[i, 1])
        f = swiglu(x, ffn_w_gate[i, 1], ffn_w_up[i, 1], ffn_w_down[i, 1])
        x = layer_norm(ALPHA * x + 0.5 * f, ln_g[i, 2], ln_b[i, 2])
    return x
```

```python
import math
from contextlib import ExitStack

import numpy as np
import concourse.bass as bass
import concourse.mybir as mybir
from concourse.bass_utils import run_bass_kernel_spmd

F32 = mybir.dt.float32
BF16 = mybir.dt.bfloat16
I32 = mybir.dt.int32
AF = mybir.ActivationFunctionType
ALU = mybir.AluOpType
AX = mybir.AxisListType

D = 1024
SEQ = 4096
NB = 8
DFF = 2816
KD = D // 128
KF = DFF // 128
DEPTH = 2
ALPHA = float((2 * DEPTH) ** 0.25)
LN_EPS = 1e-5
GN_EPS = 1e-6
CELL = 256
SLOT_BYTES = 8192
NSLOT = 4
GAMMAS = [1.0 - 2.0 ** (-5.0 - h) for h in range(4)]
LOG_GAMMAS = [math.log1p(-(2.0 ** (-5.0 - h))) for h in range(4)]


class Tl:
    def __init__(self, ap, space, c0, c1):
        self.ap = ap
        self.space = space
        self.c0 = c0
        self.c1 = c1

    def cells(self):
        return [(self.space, c) for c in range(self.c0, self.c1)]


class Eng:
    def __init__(self, prog, name, is_pe=False):
        self.prog = prog
        self.name = name
        self.is_pe = is_pe
        self.q = []
        self.sem = prog.new_sem(name)
        self.cnt = 0
        self.known = {}


class Prog:
    def __init__(self, nc, stack):
        self.nc = nc
        self.stack = stack
        self.sems = []
        self.cells = {}
        self.pe_sems = set()
        self.pe = Eng(self, "pe", True)
        self.pe_sems.add(self.pe.sem)
        self.act = Eng(self, "act")
        self.dve = Eng(self, "dve")
        self.pool = Eng(self, "pool")
        self.sp = Eng(self, "sp")
        self.dma_cnt = {}
        self.arena_bytes = 206 * 1024
        self.arena = stack.enter_context(nc.sbuf_tensor("arena", [128, self.arena_bytes // 4], F32))
        self.sp_off = 0
        self.psum = stack.enter_context(nc.psum_tensor("ps", [128, 8, 512], F32))

    def new_sem(self, name):
        h = self.stack.enter_context(self.nc.semaphore("s%d_%s" % (len(self.sems), name)))
        self.sems.append(h)
        return len(self.sems) - 1

    def alloc(self, nbytes):
        nbytes = (nbytes + CELL - 1) // CELL * CELL
        off = self.sp_off
        self.sp_off += nbytes
        self.hwm = max(getattr(self, "hwm", 0), self.sp_off)
        assert self.sp_off <= self.arena_bytes, "arena overflow %d" % self.sp_off
        return off

    def view(self, off, nelem, dt):
        esz = 4 if dt in (F32, I32) else 2
        nb = nelem * esz
        assert off % 4 == 0 and nb % 4 == 0
        ap = self.arena[:, off // 4:(off + nb) // 4]
        if dt != F32:
            ap = ap.bitcast(dt)
        return Tl(ap, "sb", off // CELL, (off + nb + CELL - 1) // CELL)

    def tile(self, nelem, dt):
        esz = 4 if dt in (F32, I32) else 2
        return self.view(self.alloc(nelem * esz), nelem, dt)

    def sub(self, tl, lo, hi, dt):
        esz = 4 if dt in (F32, I32) else 2
        base = tl.c0 * CELL
        return Tl(tl.ap[:, lo:hi], "sb", (base + lo * esz) // CELL, (base + hi * esz + CELL - 1) // CELL)

    def bank(self, b, lo=0, hi=512):
        return Tl(self.psum[:, b, lo:hi], "ps", b, b + 1)

    def op(self, eng, fn, reads=(), writes=(), signal=True, dma_sem=None):
        deps = {}
        cells = self.cells
        ps_reads = [t for t in reads if t.space == "ps"]
        if ps_reads:
            reads = [t for t in reads if t.space != "ps"]
            writes = list(writes) + ps_reads
        for t in reads:
            for c in t.cells():
                st = cells.get(c)
                if st is not None and st[0] is not None:
                    s, v = st[0]
                    if deps.get(s, 0) < v:
                        deps[s] = v
        for t in writes:
            for c in t.cells():
                st = cells.get(c)
                if st is not None:
                    if st[0] is not None:
                        s, v = st[0]
                        if deps.get(s, 0) < v:
                            deps[s] = v
                    for s, v in st[1].items():
                        if deps.get(s, 0) < v:
                            deps[s] = v
        waits = []
        for s, v in deps.items():
            if eng.is_pe and s in self.pe_sems:
                continue
            if eng.known.get(s, 0) >= v:
                continue
            eng.known[s] = v
            waits.append((s, v))
        if dma_sem is not None:
            self.dma_cnt[dma_sem] = self.dma_cnt.get(dma_sem, 0) + 16
            evt = (dma_sem, self.dma_cnt[dma_sem])
            inc = (dma_sem, 16)
        else:
            evt = (eng.sem, eng.cnt + 1)
            if signal:
                eng.cnt += 1
                inc = (eng.sem, 1)
            else:
                inc = None
        for t in reads:
            for c in t.cells():
                st = cells.get(c)
                if st is None:
                    st = cells[c] = [None, {}]
                if st[1].get(evt[0], 0) < evt[1]:
                    st[1][evt[0]] = evt[1]
        for t in writes:
            for c in t.cells():
                cells[c] = [evt, {}]
        eng.q.append((waits, fn, inc))
        return evt

    def emit(self):
        nc = self.nc
        sems = self.sems

        def run(q):
            def body(e):
                for waits, fn, inc in q:
                    for s, v in waits:
                        e.wait_ge(sems[s], v)
                    ins = fn(e)
                    if inc is not None:
                        ins.then_inc(sems[inc[0]], inc[1])
            return body

        with nc.Block() as block:
            block.tensor(run(self.pe.q))
            block.scalar(run(self.act.q))
            block.vector(run(self.dve.q))
            block.gpsimd(run(self.pool.q))
            block.sync(run(self.sp.q))


class Kern:
    def __init__(self, nsub=6, ntiles=None, T=512):
        self.T = T
        self.NG = T // 512
        self.ntiles = ntiles if ntiles is not None else SEQ // T
        self.nsub = nsub
        self.nc = nc = bass.Bass("TRN2", target_bir_lowering=False)
        dt = nc.dram_tensor
        self.x = dt("x", [SEQ, D], F32, kind="ExternalInput").ap()
        self.ln_g = dt("ln_g", [2, 3, D], F32, kind="ExternalInput").ap()
        self.ln_b = dt("ln_b", [2, 3, D], F32, kind="ExternalInput").ap()
        self.wg = dt("ffn_w_gate", [2, 2, D, DFF], F32, kind="ExternalInput").ap()
        self.wu = dt("ffn_w_up", [2, 2, D, DFF], F32, kind="ExternalInput").ap()
        self.wd = dt("ffn_w_down", [2, 2, DFF, D], F32, kind="ExternalInput").ap()
        self.a_w_in = dt("a_w_in", [1, D, 4096], F32, kind="ExternalInput").ap()
        self.a_ln_g = dt("a_ln_g", [1, 2048], F32, kind="ExternalInput").ap()
        self.a_ln_b = dt("a_ln_b", [1, 2048], F32, kind="ExternalInput").ap()
        self.a_w_s = dt("a_w_s", [1, 8, 128, 128], F32, kind="ExternalInput").ap()
        self.a_b_s = dt("a_b_s", [1, 8, 128], F32, kind="ExternalInput").ap()
        self.a_w_out = dt("a_w_out", [1, 2048, D], F32, kind="ExternalInput").ap()
        self.b_w_in = dt("b_w_in", [1, D, 6144], F32, kind="ExternalInput").ap()
        self.b_w_out = dt("b_w_out", [1, 2048, D], F32, kind="ExternalInput").ap()
        self.out = dt("out", [SEQ, D], F32, kind="ExternalOutput").ap()

    def mm(self, out, lhsT, rhs, start, stop, reads, signal=None):
        P = self.P
        if signal is None:
            signal = stop
        o, l, r = out.ap, lhsT, rhs
        P.op(P.pe, lambda e: e.matmul(o, l, r, start=start, stop=stop), reads=reads, writes=[out], signal=signal)

    def act_fn(self, out, in_, func, reads=None, bias=None, scale=None, extra_reads=(), accum=None):
        P = self.P
        kw = {}
        if bias is not None:
            kw["bias"] = bias
        if scale is not None:
            kw["scale"] = scale
        wr = [out]
        if accum is not None:
            kw["accum_out"] = accum.ap
            wr.append(accum)
        o, i = out.ap, in_.ap
        P.op(P.act, lambda e: e.activation(o, i, func, **kw), reads=[in_] + list(extra_reads), writes=wr)

    def tt(self, out, in0, in1, op, eng=None):
        P = self.P
        eng = eng or P.dve
        o, a, b = out.ap, in0.ap, in1.ap
        P.op(eng, lambda e: e.tensor_tensor(o, a, b, op), reads=[in0, in1], writes=[out])

    def ts(self, out, in0, s1, s2, op0, op1=None, extra_reads=(), eng=None, accum=None):
        P = self.P
        eng = eng or P.dve
        o, a = out.ap, in0.ap
        wr = [out]
        kw = {}
        if accum is not None:
            kw["accum_out"] = accum.ap
            wr.append(accum)
        if op1 is None:
            P.op(eng, lambda e: e.tensor_scalar(o, a, s1, None, op0, **kw), reads=[in0] + list(extra_reads), writes=wr)
        else:
            P.op(eng, lambda e: e.tensor_scalar(o, a, s1, s2, op0, op1, **kw), reads=[in0] + list(extra_reads), writes=wr)

    def stt(self, out, in0, scalar, in1, op0, op1, extra_reads=(), accum=None):
        P = self.P
        o, a, b = out.ap, in0.ap, in1.ap
        wr = [out]
        kw = {}
        if accum is not None:
            kw["accum_out"] = accum.ap
            wr.append(accum)
        P.op(P.dve, lambda e: e.scalar_tensor_tensor(o, a, scalar, b, op0, op1, **kw),
             reads=[in0, in1] + list(extra_reads), writes=wr)

    def wload(self, src_ap, shape):
        P = self.P
        s = self.slot_i % NSLOT
        self.slot_i += 1
        n = 1
        for d in shape[1:]:
            n *= d
        assert n * 2 <= SLOT_BYTES
        full = self.slots[s]
        v = full.ap[:, 0:n]
        if len(shape) == 3:
            v = v.rearrange("p (a b) -> p a b", b=shape[2])
        tl = Tl(v, "sb", full.c0, full.c1)
        P.op(P.pool, lambda e: e.dma_start(out=v, in_=src_ap), reads=[], writes=[tl], dma_sem=self.slot_sems[s])
        return tl

    def build(self):
        nc = self.nc
        with ExitStack() as stack:
            self.P = P = Prog(nc, stack)
            T, NG = self.T, self.NG
            self.slots = [P.tile(SLOT_BYTES // 2, BF16) for _ in range(NSLOT)]
            self.slot_sems = [P.new_sem("slot%d" % i) for i in range(NSLOT)]
            self.slot_i = 0
            self.in_sems = [P.new_sem("in%d" % i) for i in range(4)]
            self.out_sems = [P.new_sem("out%d" % i) for i in range(4)]
            self.c_sem = P.new_sem("const")
            self.xres = [[P.tile(512, F32) for g in range(NG)] for dc in range(KD)]
            self.xop = [[P.tile(512, BF16) for g in range(NG)] for dc in range(KD)]
            self.ident = P.tile(128, F32)
            self.ones_bf = P.tile(128, BF16)
            self.cst = P.tile(64, F32)
            self.dummy = P.tile(64, F32)
            self.next_func = None
            self.pending = []
            self.last_sub = False
            self.cst_cols = {}
            self.lngb = P.tile(12 * KD, F32)
            self.lng = P.sub(self.lngb, 0, 6 * KD, F32)
            self.lnb = P.sub(self.lngb, 6 * KD, 12 * KD, F32)
            self.m_t = P.tile(512, F32)
            self.v_t = P.tile(512, F32)
            self.r_t = P.tile(512, F32)
            self.sg_t = [P.tile(512, F32) for _ in range(2)]
            self.sg_i = 0
            self.scratch_base = P.sp_off
            self.mm_banks = [0, 1, 2, 3]
            self.mm_i = 0
            self.y_banks = [4, 5]
            self.y_i = 0

            self.setup_consts()
            if self.nsub >= 2:
                self.setup_mixer_a()
            if self.nsub >= 5:
                self.setup_mixer_b()
            self.scratch_base = P.sp_off
            self.load_dma(0)
            for ti in range(self.ntiles):
                self.load_tile(ti)
                if self.nsub >= 5 and ti > 0:
                    self.rotary_tables(ti)
                si = 0
                for li in range(DEPTH):
                    for part in range(3):
                        if si >= self.nsub:
                            break
                        last = (si == self.nsub - 1)
                        if last and ti + 1 < self.ntiles and part != 1:
                            self.load_dma(ti + 1)
                        self.next_func = AF.Gelu if (li == 0 and part == 0) else AF.Silu
                        self.last_sub = last
                        if part == 0:
                            self.ffn(li, 0, li * 3 + 0)
                        elif part == 1:
                            if li == 0:
                                if ti == 0:
                                    self.emit_mixer_a()
                                self.mixer_a(li * 3 + 1, ti)
                            else:
                                if ti == 0:
                                    self.emit_mixer_b()
                                    self.rotary_tables(0)
                                self.mixer_b(li * 3 + 1, ti)
                        else:
                            self.ffn(li, 1, li * 3 + 2)
                        si += 1
                if ti + 1 < self.ntiles and (self.nsub == 0 or (self.nsub - 1) % 3 == 1):
                    self.load_dma(ti + 1)
                self.store_tile(ti)
            P.sp.q.append(([(s_, P.dma_cnt.get(s_, 0)) for s_ in self.out_sems], lambda e: e.nop(), None))
            P.emit()
        return nc

    def kouter(self, accs):
        for k in range(KD):
            for (bk, lf, rf, rdf) in accs:
                self.mm(bk, lf(k), rf(k), k == 0, k == KD - 1, reads=rdf(k))

    def next_mm_bank(self):
        b = self.mm_banks[self.mm_i % len(self.mm_banks)]
        self.mm_i += 1
        return b

    def next_y_bank(self):
        b = self.y_banks[self.y_i % len(self.y_banks)]
        self.y_i += 1
        return b

    def setup_consts(self):
        P = self.P
        nc = self.nc
        idt = self.ident
        P.op(P.pool, lambda e: e.memset(idt.ap, 1.0), writes=[idt])
        P.op(P.pool, lambda e: e.affine_select(idt.ap, idt.ap, [[1, 128]], ALU.is_equal, 0.0, base=0,
                                               channel_multiplier=-1), reads=[idt], writes=[idt])
        ob = self.ones_bf
        P.op(P.pool, lambda e: e.memset(ob.ap, 1.0), writes=[ob])
        for l in range(2):
            for j in range(3):
                idx = (l * 3 + j) * KD
                for (dst, src) in ((self.lng, self.ln_g), (self.lnb, self.ln_b)):
                    d_ap = dst.ap[:, idx:idx + KD]
                    s_ap = src[l, j].rearrange("(c p) -> p c", p=128)
                    P.op(P.sp, lambda e, d_ap=d_ap, s_ap=s_ap: e.dma_start(out=d_ap, in_=s_ap,
                                                                          allow_slow_non_contiguous=True),
                         writes=[dst], dma_sem=self.c_sem)

    def load_dma(self, ti):
        P = self.P
        self.in_st = []
        for g in range(self.NG):
            st = [P.view(self.scratch_base + 40960 + (g * 4 + c) * 4096, D, F32) for c in range(4)]
            self.in_st.append(st)
            for c in range(4):
                r0 = ti * self.T + g * 512 + c * 128
                src = self.x[r0:r0 + 128, :]
                d_ap = st[c].ap
                P.op(P.sp, lambda e, d_ap=d_ap, src=src: e.dma_start(out=d_ap, in_=src), writes=[st[c]],
                     dma_sem=self.in_sems[c])

    def load_tile(self, ti):
        P = self.P
        for g in range(self.NG):
            st = self.in_st[g]
            for dc in range(KD):
                b = self.next_mm_bank()
                for c in range(4):
                    o = P.bank(b, c * 128, (c + 1) * 128)
                    i_ap = st[c].ap[:, dc * 128:(dc + 1) * 128]
                    idt = self.ident
                    P.op(P.pe, lambda e, o=o, i_ap=i_ap, idt=idt: e.transpose(o.ap, i_ap, idt.ap),
                         reads=[st[c], idt], writes=[o], signal=(c == 3))
                bk = P.bank(b)
                xr, xo = self.xres[dc][g], self.xop[dc][g]
                P.op(P.act, lambda e, xr=xr, bk=bk: e.activation(xr.ap, bk.ap, AF.Copy), reads=[bk], writes=[xr])
                P.op(P.dve, lambda e, xo=xo, xr=xr: e.tensor_copy(xo.ap, xr.ap), reads=[xr], writes=[xo])

    def store_tile(self, ti):
        P = self.P
        self.flush_pending()
        mark = P.sp_off
        for g in range(self.NG):
            st = [P.tile(D, F32) for _ in range(4)]
            for c in range(4):
                for half in range(2):
                    b = self.next_mm_bank()
                    for q in range(4):
                        dc = half * 4 + q
                        o = P.bank(b, q * 128, (q + 1) * 128)
                        xr = self.xres[dc][g]
                        i_ap = xr.ap[:, c * 128:(c + 1) * 128]
                        idt = self.ident
                        P.op(P.pe, lambda e, o=o, i_ap=i_ap, idt=idt: e.transpose(o.ap, i_ap, idt.ap),
                             reads=[xr, idt], writes=[o], signal=(q == 3))
                    bk = P.bank(b)
                    dst = P.sub(st[c], half * 512, (half + 1) * 512, F32)
                    if half == 0:
                        P.op(P.act, lambda e, dst=dst, bk=bk: e.activation(dst.ap, bk.ap, AF.Copy), reads=[bk], writes=[dst])
                    else:
                        P.op(P.dve, lambda e, dst=dst, bk=bk: e.tensor_copy(dst.ap, bk.ap), reads=[bk], writes=[dst])
                r0 = ti * self.T + g * 512 + c * 128
                dst_ap = self.out[r0:r0 + 128, :]
                s_ap = st[c].ap
                P.op(P.sp, lambda e, dst_ap=dst_ap, s_ap=s_ap: e.dma_start(out=dst_ap, in_=s_ap), reads=[st[c]],
                     dma_sem=self.out_sems[c])
        P.sp_off = mark

    def ln_stats_mm(self, g, dc, s1, s2):
        self.mm(s1, self.ones_bf.ap, self.zb[dc].ap, dc == 0, dc == KD - 1, reads=[self.ones_bf, self.zb[dc]])
        self.mm(s2, self.ones_bf.ap, self.zsq[dc].ap, dc == 0, dc == KD - 1, reads=[self.ones_bf, self.zsq[dc]])

    def ln_finalize(self, g, lnidx, eps, s1, s2):
        P = self.P
        m, v, r = self.m_t, self.v_t, self.r_t
        self.ts(m, s1, 1.0 / D, None, ALU.mult)
        self.tt(v, m, m, ALU.mult)
        self.stt(v, s2, 1.0 / D, v, ALU.mult, ALU.subtract)
        self.act_fn(r, v, AF.Ln, bias=self.const_ap(eps), extra_reads=[self.cst])
        self.act_fn(r, r, AF.Exp, scale=-0.5)
        gb = []
        for dc in range(KD):
            z = self.xres[dc][g]
            self.tt(z, z, m, ALU.subtract)
            self.tt(z, z, r, ALU.mult)
            gi = lnidx * KD + dc
            g_ap = self.lng.ap[:, gi:gi + 1]
            b_ap = self.lnb.ap[:, gi:gi + 1]
            gb.append((g_ap, b_ap))
            if self.last_sub:
                self.act_fn(z, z, AF.Identity, bias=b_ap, scale=g_ap, extra_reads=[self.lng, self.lnb])
                continue
            self.act_fn(self.xop[dc][g], z, AF.Identity, bias=b_ap, scale=g_ap, extra_reads=[self.lng, self.lnb])
        if self.last_sub:
            return
        if self.next_func is not None:
            self.act_fn(P.sub(self.dummy, 0, 1, F32), self.const_tl(1.0), self.next_func)

        def finish(dc_list=tuple(range(KD)), g=g, gb=gb):
            for dc in dc_list:
                z = self.xres[dc][g]
                g_ap, b_ap = gb[dc]
                self.act_fn(z, z, AF.Identity, bias=b_ap, scale=g_ap, extra_reads=[self.lng, self.lnb])

        self.pending.append(finish)

    def flush_pending(self):
        pend, self.pending = self.pending, []
        for f in pend:
            f()

    def const_tl(self, val):
        self.const_ap(val)
        i = self.cst_cols[val]
        return self.P.sub(self.cst, i, i + 1, F32)

    def const_ap(self, val):
        P = self.P
        if val not in self.cst_cols:
            i = len(self.cst_cols)
            assert i < 16
            self.cst_cols[val] = i
            col = self.cst.ap[:, i:i + 1]
            P.op(P.pool, lambda e: e.memset(col, float(val)), writes=[self.cst])
        i = self.cst_cols[val]
        return self.cst.ap[:, i:i + 1]

    def resid_ln(self, lnidx, c_res, eps, ybank_fn, zoff=None):
        P = self.P
        self.flush_pending()
        if zoff is None:
            self.zb = [P.tile(512, BF16) for _ in range(KD)]
            self.zsq = [P.tile(512, BF16) for _ in range(KD)]
        else:
            self.zb = [P.view(zoff + i * 1024, 512, BF16) for i in range(KD)]
            self.zsq = [P.view(zoff + 8192 + i * 1024, 512, BF16) for i in range(KD)]
        for g in range(self.NG):
            s1, s2 = P.bank(6), P.bank(7)
            prev = None
            self.act_fn(P.sub(self.dummy, 0, 1, F32), self.const_tl(1.0), AF.Ln, bias=self.const_ap(1.0), extra_reads=[self.cst])
            for dc in range(KD):
                bk = P.bank(self.next_y_bank())
                ybank_fn(dc, g, bk)
                if prev is not None:
                    self.ln_stats_mm(g, prev, s1, s2)
                z = self.xres[dc][g]
                self.stt(z, z, c_res, bk, ALU.mult, ALU.add)
                self.act_fn(self.zb[dc], z, AF.Copy)
                self.act_fn(self.zsq[dc], z, AF.Square)
                prev = dc
            self.ln_stats_mm(g, prev, s1, s2)
            self.ln_finalize(g, lnidx, eps, s1, s2)

    def ffn(self, li, j, lnidx):
        P = self.P
        NG = self.NG
        mark = P.sp_off
        h = [[P.tile(512, BF16) for g in range(NG)] for f in range(KF)]
        wg = self.wg[li, j].rearrange("(k p) f -> p k f", p=128)
        wu = self.wu[li, j].rearrange("(k p) f -> p k f", p=128)
        wd = self.wd[li, j].rearrange("(f p) d -> p f d", p=128)
        first = True
        for c0 in range(0, DFF, 512):
            n = min(512, DFF - c0)
            sg = self.wload(wg[:, :, c0:c0 + n], [128, KD, n])
            su = self.wload(wu[:, :, c0:c0 + n], [128, KD, n])
            fls = list(range(n // 128))
            if first and NG == 1:
                first = False
                g = 0
                accs = []
                for fl in fls[:3]:
                    for (w, b) in ((sg, 2 * fl), (su, 2 * fl + 1)):
                        accs.append((P.bank(b),
                                     (lambda k, w=w, fl=fl: w.ap[:, k, fl * 128:(fl + 1) * 128]),
                                     (lambda k: self.xop[k][g].ap),
                                     (lambda k, w=w: [w, self.xop[k][g]])))
                self.kouter(accs)
                for fl in fls[:3]:
                    tmp = self.sg_t[self.sg_i % 2]
                    self.sg_i += 1
                    self.act_fn(tmp, P.bank(2 * fl), AF.Silu)
                    self.tt(h[c0 // 128 + fl][g], tmp, P.bank(2 * fl + 1), ALU.mult)
                self.flush_pending()
                fls = fls[3:]
            for fl in fls:
                fc = c0 // 128 + fl
                for g in range(NG):
                    bg = P.bank(self.next_mm_bank())
                    bu = P.bank(self.next_mm_bank())
                    for k in range(KD):
                        self.mm(bg, sg.ap[:, k, fl * 128:(fl + 1) * 128], self.xop[k][g].ap, k == 0, k == KD - 1,
                                reads=[sg, self.xop[k][g]])
                    for k in range(KD):
                        self.mm(bu, su.ap[:, k, fl * 128:(fl + 1) * 128], self.xop[k][g].ap, k == 0, k == KD - 1,
                                reads=[su, self.xop[k][g]])
                    tmp = self.sg_t[self.sg_i % 2]
                    self.sg_i += 1
                    self.act_fn(tmp, bg, AF.Silu)
                    self.tt(h[fc][g], tmp, bu, ALU.mult)

        wd_slots = {}

        def yfn(dc, g, bk):
            if g == 0 or dc not in wd_slots:
                wd_slots[dc] = self.wload(wd[:, :, dc * 128:(dc + 1) * 128], [128, KF, 128])
            sd = wd_slots[dc]
            for f in range(KF):
                self.mm(bk, sd.ap[:, f, :], h[f][g].ap, f == 0, f == KF - 1, reads=[sd, h[f][g]])

        self.resid_ln(lnidx, 2.0 * ALPHA, 4.0 * LN_EPS, yfn)
        P.sp_off = mark

    def setup_mixer_a(self):
        P = self.P
        self.a_wsT = P.tile(8 * 128, BF16)
        self.a_blhs = P.tile(2048, BF16)
        self.a_brhs = P.tile(1024, BF16)
        self.a_G = P.tile(2048, F32)
        self.a_sem = P.new_sem("a_const")

    def emit_mixer_a(self):
        P = self.P
        mark = P.sp_off
        wst = P.tile(1024, F32)
        wT = P.tile(1024, F32)
        bs_f = P.tile(1024, F32)
        bs_h = P.tile(1024, BF16)
        bs_l = P.tile(1024, BF16)
        ones_f = P.tile(1, F32)
        csem = self.a_sem
        src = self.a_w_s[0].rearrange("g t s -> t g s")
        d3 = wst.ap.rearrange("p (g s) -> p g s", s=128)
        P.op(P.sp, lambda e: e.dma_start(out=d3, in_=src), writes=[wst], dma_sem=P.new_sem("ac"))
        P.op(P.pool, lambda e: e.memset(ones_f.ap, 1.0), writes=[ones_f])
        for half in range(2):
            b = self.next_mm_bank()
            for q in range(4):
                g = half * 4 + q
                o = P.bank(b, q * 128, (q + 1) * 128)
                i_ap = wst.ap[:, g * 128:(g + 1) * 128]
                idt = self.ident
                P.op(P.pe, lambda e, o=o, i_ap=i_ap, idt=idt: e.transpose(o.ap, i_ap, idt.ap), reads=[wst, idt],
                     writes=[o], signal=(q == 3))
            bk = P.bank(b)
            dst = P.sub(wT, half * 512, (half + 1) * 512, F32)
            P.op(P.act, lambda e, dst=dst, bk=bk: e.activation(dst.ap, bk.ap, AF.Copy), reads=[bk], writes=[dst])
        P.op(P.pool, lambda e: e.affine_select(wT.ap, wT.ap, [[0, 8], [1, 128]], ALU.is_ge, 0.0, base=0,
                                               channel_multiplier=-1), reads=[wT], writes=[wT])
        wsT = self.a_wsT
        P.op(P.dve, lambda e: e.tensor_copy(wsT.ap, wT.ap), reads=[wT], writes=[wsT])
        brhs = self.a_brhs
        for half in range(2):
            bk = P.bank(self.next_mm_bank())
            o_ap = bk.ap[0:1, :]
            r_ap = wT.ap[:, half * 512:(half + 1) * 512]
            l_ap = ones_f.ap
            P.op(P.pe, lambda e, o_ap=o_ap, l_ap=l_ap, r_ap=r_ap: e.matmul(o_ap, l_ap, r_ap, start=True, stop=True),
                 reads=[ones_f, wT], writes=[bk])
            d_ap = brhs.ap[0:1, half * 512:(half + 1) * 512]
            P.op(P.act, lambda e, d_ap=d_ap, o_ap=o_ap: e.activation(d_ap, o_ap, AF.Copy), reads=[bk], writes=[brhs])
        bsrc = self.a_b_s[0:1].rearrange("o g t -> o (g t)")
        P.op(P.sp, lambda e: e.dma_start(out=bs_f.ap[0:1, :], in_=bsrc), writes=[bs_f], dma_sem=P.new_sem("ac"))
        P.op(P.dve, lambda e: e.tensor_copy(bs_h.ap[0:1, :], bs_f.ap[0:1, :]), reads=[bs_f], writes=[bs_h])
        P.op(P.dve, lambda e: e.tensor_tensor(bs_l.ap[0:1, :], bs_f.ap[0:1, :], bs_h.ap[0:1, :], ALU.subtract),
             reads=[bs_f, bs_h], writes=[bs_l])
        P.op(P.sp, lambda e: e.dma_start(out=brhs.ap[1:2, :], in_=bs_h.ap[0:1, :]), reads=[bs_h], writes=[brhs],
             dma_sem=P.new_sem("ac"))
        P.op(P.sp, lambda e: e.dma_start(out=brhs.ap[2:3, :], in_=bs_l.ap[0:1, :]), reads=[bs_l], writes=[brhs],
             dma_sem=P.new_sem("ac"))
        blhs = self.a_blhs
        P.op(P.pool, lambda e: e.memset(blhs.ap[0:3, :], 1.0), writes=[blhs])
        P.op(P.pool, lambda e: e.dma_start(out=blhs.ap[0:1, :], in_=self.a_ln_b[0:1, :]), writes=[blhs],
             dma_sem=P.new_sem("ac"))
        G = self.a_G
        gsrc = self.a_ln_g[0:1, :].partition_broadcast(128)
        g3 = G.ap.rearrange("p (o f) -> p o f", o=1)
        P.op(P.sp, lambda e: e.dma_start(out=g3, in_=gsrc), writes=[G], dma_sem=P.new_sem("ac"))
        P.sp_off = mark

    def mixer_a(self, lnidx, ti):
        P = self.P
        NG = self.NG
        mark = P.sp_off
        w_in = self.a_w_in[0].rearrange("(k p) f -> p k f", p=128)
        w_out = self.a_w_out[0].rearrange("(f p) d -> p f d", p=128)
        vt_off = P.sp_off
        vt = [[P.tile(2048, F32) for c in range(4)] for g in range(NG)]
        vln = [[P.tile(2048, BF16) for c in range(4)] for g in range(NG)]
        ut = [[(P.tile(512, F32) if f < 4 else P.view(vt_off + g * 32768 + (f - 4) * 2048, 512, F32))
               for f in range(16)] for g in range(NG)]
        gated = [[P.tile(512, BF16) for g in range(NG)] for f in range(16)]
        s1 = [P.tile(16, F32) for g in range(NG)]
        s2 = [P.tile(16, F32) for g in range(NG)]
        st4 = [P.tile(16, F32) for g in range(NG)]
        junk = [P.tile(512, F32) for _ in range(2)]
        for vg in range(4):
            wb = self.wload(w_in[:, :, 2048 + vg * 512:2048 + (vg + 1) * 512], [128, KD, 512])
            for g in range(NG):
                kout = (vg == 0 and NG == 1)
                if kout:
                    self.kouter([(P.bank(c),
                                  (lambda k, c=c: self.xop[k][g].ap[:, c * 128:(c + 1) * 128]),
                                  (lambda k, wb=wb: wb.ap[:, k, :]),
                                  (lambda k, wb=wb: [wb, self.xop[k][g]])) for c in range(4)])
                for c in range(4):
                    if kout:
                        bk = P.bank(c)
                    else:
                        bk = P.bank(self.next_mm_bank())
                        for k in range(KD):
                            self.mm(bk, self.xop[k][g].ap[:, c * 128:(c + 1) * 128], wb.ap[:, k, :], k == 0, k == KD - 1,
                                    reads=[wb, self.xop[k][g]])
                    vsl = P.sub(vt[g][c], vg * 512, (vg + 1) * 512, F32)
                    a1 = P.sub(s1[g], c * 4 + vg, c * 4 + vg + 1, F32)
                    self.act_fn(vsl, bk, AF.Gelu, accum=a1)
                    a2 = P.sub(s2[g], c * 4 + vg, c * 4 + vg + 1, F32)
                    jk = junk[(c + vg) % 2]
                    self.stt(jk, vsl, 1.0, vsl, ALU.mult, ALU.mult, accum=a2)
        self.flush_pending()
        for ug in range(4):
            wb = self.wload(w_in[:, :, ug * 512:(ug + 1) * 512], [128, KD, 512])
            for fl in range(4):
                fc = ug * 4 + fl
                for g in range(NG):
                    bk = P.bank(self.next_mm_bank())
                    for k in range(KD):
                        self.mm(bk, wb.ap[:, k, fl * 128:(fl + 1) * 128], self.xop[k][g].ap, k == 0, k == KD - 1,
                                reads=[wb, self.xop[k][g]])
                    self.act_fn(ut[g][fc], bk, AF.Gelu)
            if ug == 0:
                for g in range(NG):
                    st = st4[g]
                    mean = P.sub(st, 0, 4, F32)
                    e2 = P.sub(st, 4, 8, F32)
                    var = P.sub(st, 8, 12, F32)
                    tmp = P.sub(st, 12, 16, F32)
                    s1v = s1[g].ap.rearrange("p (c v) -> p c v", v=4)
                    s2v = s2[g].ap.rearrange("p (c v) -> p c v", v=4)
                    P.op(P.dve, lambda e, mean=mean, s1v=s1v: e.tensor_reduce(mean.ap, s1v, AX.X, ALU.add),
                         reads=[s1[g]], writes=[mean])
                    P.op(P.dve, lambda e, e2=e2, s2v=s2v: e.tensor_reduce(e2.ap, s2v, AX.X, ALU.add),
                         reads=[s2[g]], writes=[e2])
                    self.ts(mean, mean, 1.0 / 2048, None, ALU.mult)
                    self.tt(tmp, mean, mean, ALU.mult)
                    self.stt(var, e2, 1.0 / 2048, tmp, ALU.mult, ALU.subtract)
                    self.act_fn(var, var, AF.Sqrt, bias=self.const_ap(LN_EPS), extra_reads=[self.cst])
                    P.op(P.dve, lambda e, var=var: e.reciprocal(var.ap, var.ap), reads=[var], writes=[var])
                    for c in range(4):
                        m_ap = mean.ap[:, c:c + 1]
                        r_ap = var.ap[:, c:c + 1]
                        self.ts(vt[g][c], vt[g][c], m_ap, r_ap, ALU.subtract, ALU.mult, extra_reads=[st])
                        self.tt(vln[g][c], vt[g][c], self.a_G, ALU.mult)
        for fc in range(16):
            G8 = fc // 2
            for g in range(NG):
                bk = P.bank(self.next_mm_bank())
                for c in range(4):
                    o = P.bank(bk.c0, c * 128, (c + 1) * 128)
                    self.mm(o, vln[g][c].ap[:, fc * 128:(fc + 1) * 128], self.a_wsT.ap[:, G8 * 128:(G8 + 1) * 128],
                            True, False, reads=[vln[g][c], self.a_wsT], signal=False)
                    self.mm(o, self.a_blhs.ap[0:3, fc * 128:(fc + 1) * 128], self.a_brhs.ap[0:3, G8 * 128:(G8 + 1) * 128],
                            False, True, reads=[self.a_blhs, self.a_brhs], signal=(c == 3))
                self.tt(gated[fc][g], ut[g][fc], bk, ALU.mult)
        wo_slots = {}

        def yfn(dc, g, bk):
            if g == 0 or dc not in wo_slots:
                wo_slots[dc] = self.wload(w_out[:, :, dc * 128:(dc + 1) * 128], [128, 16, 128])
            sd = wo_slots[dc]
            for f in range(16):
                self.mm(bk, sd.ap[:, f, :], gated[f][g].ap, f == 0, f == 15, reads=[sd, gated[f][g]])

        self.resid_ln(lnidx, ALPHA, LN_EPS, yfn, zoff=vt_off + 24576 if NG == 1 else None)
        P.sp_off = mark

    def setup_mixer_b(self):
        P = self.P
        self.b_DT = P.tile(512, F32)
        self.b_qdec = P.tile(512, F32)
        self.b_kdec = P.tile(4, F32)
        self.b_pos0 = P.tile(512, F32)
        self.b_inv = P.tile(1, F32)
        self.ident_bf = P.tile(128, BF16)
        self.rot_tabs = [P.tile(512, F32) for _ in range(4)]
        self.S = [[P.tile(512, F32) for half in range(2)] for h in range(4)]
        self.Sbf = [[P.tile(512, BF16) for half in range(2)] for h in range(4)]

    def emit_mixer_b(self):
        P = self.P
        mark = P.sp_off
        rel = P.tile(128, F32)
        t1 = P.tile(128, F32)
        k1 = P.tile(1, F32)
        pidx = P.tile(1, F32)
        idb, idf = self.ident_bf, self.ident
        P.op(P.dve, lambda e: e.tensor_copy(idb.ap, idf.ap), reads=[idf], writes=[idb])
        P.op(P.pool, lambda e: e.iota(rel.ap, [[1, 128]], base=0, channel_multiplier=-1,
                                      allow_small_or_imprecise_dtypes=True), writes=[rel])
        P.op(P.pool, lambda e: e.iota(t1.ap, [[1, 128]], base=1, channel_multiplier=0,
                                      allow_small_or_imprecise_dtypes=True), writes=[t1])
        P.op(P.pool, lambda e: e.iota(k1.ap, [[0, 1]], base=127, channel_multiplier=-1,
                                      allow_small_or_imprecise_dtypes=True), writes=[k1])
        P.op(P.pool, lambda e: e.iota(pidx.ap, [[0, 1]], base=0, channel_multiplier=1,
                                      allow_small_or_imprecise_dtypes=True), writes=[pidx])
        pos0 = self.b_pos0
        P.op(P.pool, lambda e: e.iota(pos0.ap, [[1, 512]], base=0, channel_multiplier=0,
                                      allow_small_or_imprecise_dtypes=True), writes=[pos0])
        for h in range(4):
            lg = LOG_GAMMAS[h]
            self.act_fn(P.sub(self.b_DT, h * 128, (h + 1) * 128, F32), rel, AF.Exp, scale=lg)
            self.act_fn(P.sub(self.b_qdec, h * 128, (h + 1) * 128, F32), t1, AF.Exp, scale=lg)
            self.act_fn(P.sub(self.b_kdec, h, h + 1, F32), k1, AF.Exp, scale=lg)
        DT = self.b_DT
        P.op(P.pool, lambda e: e.affine_select(DT.ap, DT.ap, [[0, 4], [1, 128]], ALU.is_ge, 0.0, base=0,
                                               channel_multiplier=-1), reads=[DT], writes=[DT])
        self.act_fn(self.b_inv, pidx, AF.Exp, scale=-math.log(10000.0) / 128.0)
        for h in range(4):
            for half in range(2):
                S, Sb = self.S[h][half], self.Sbf[h][half]
                P.op(P.pool, lambda e, S=S: e.memset(S.ap, 0.0), writes=[S])
                P.op(P.pool, lambda e, Sb=Sb: e.memset(Sb.ap, 0.0), writes=[Sb])
        P.sp_off = mark

    def rotary_tables(self, ti):
        P = self.P
        PI = math.pi
        TWO_PI = 2.0 * math.pi
        C1 = 6.28125
        C2 = TWO_PI - C1
        cos_t, sin_t, cos16, sin16 = self.rot_tabs
        mark = P.sp_off
        ang, kf, tfix, rc = (P.tile(512, F32) for _ in range(4))
        kfi = P.tile(512, I32)
        base = ti * self.T
        self.ts(ang, self.b_pos0, float(base), self.b_inv.ap[:, 0:1], ALU.add, ALU.mult, extra_reads=[self.b_inv])
        self.ts(kfi, ang, 1.0 / TWO_PI, None, ALU.mult)
        P.op(P.dve, lambda e: e.tensor_copy(kf.ap, kfi.ap), reads=[kfi], writes=[kf])
        self.stt(ang, kf, -C1, ang, ALU.mult, ALU.add)
        self.stt(ang, kf, -C2, ang, ALU.mult, ALU.add)
        self.ts(tfix, ang, PI, TWO_PI, ALU.is_gt, ALU.mult)
        self.tt(ang, ang, tfix, ALU.subtract)
        self.ts(tfix, ang, -PI, TWO_PI, ALU.is_lt, ALU.mult)
        self.tt(ang, ang, tfix, ALU.add)
        self.ts(rc, ang, PI / 2.0, None, ALU.add)
        self.ts(tfix, rc, PI, TWO_PI, ALU.is_gt, ALU.mult)
        self.tt(rc, rc, tfix, ALU.subtract)
        self.act_fn(sin_t, ang, AF.Sin)
        self.act_fn(cos_t, rc, AF.Sin)
        self.ts(sin16, sin_t, 1.0 / 16.0, None, ALU.mult)
        self.ts(cos16, cos_t, 1.0 / 16.0, None, ALU.mult)
        P.sp_off = mark

    def mixer_b(self, lnidx, ti):
        P = self.P
        NG = self.NG
        mark = P.sp_off
        w_in = self.b_w_in[0].rearrange("(k p) f -> p k f", p=128)
        w_out = self.b_w_out[0].rearrange("(f p) d -> p f d", p=128)
        PI = math.pi
        TWO_PI = 2.0 * math.pi
        C1 = 6.28125
        C2 = TWO_PI - C1
        pbanks = [0, 1, 2]

        def pbank():
            b = pbanks[self.mm_i % 3]
            self.mm_i += 1
            return b

        z_off = P.sp_off
        cos_t, sin_t, cos16, sin16 = self.rot_tabs
        ang, kf, tfix, rc = (P.tile(512, F32) for _ in range(4))
        ta, tb, tc, td = ang, kf, tfix, rc
        qT = [P.tile(512, BF16) for _ in range(2)]
        kT = [P.tile(512, BF16) for _ in range(2)]
        qd = [P.tile(512, BF16) for _ in range(2)]
        vh = [P.tile(512, BF16) for _ in range(4)]
        kd = [P.tile(256, BF16) for _ in range(4)]
        sc = [P.tile(128, BF16) for _ in range(2)]
        yh = P.tile(2048, F32)
        yb = [P.tile(512, BF16) for _ in range(4)]
        ysq = [P.tile(512, BF16) for _ in range(4)]
        gated = [[P.tile(512, BF16) for g in range(NG)] for f in range(16)]
        sgh = [P.tile(512, F32) for _ in range(4)]
        sci = 0
        for g in range(NG):
            def do_qk(h):
                qdec_b = self.b_qdec.ap[:, h * 128:(h + 1) * 128].unsqueeze(1).broadcast_to([128, 4, 128])
                wq = self.wload(w_in[:, :, h * 256:h * 256 + 256], [128, KD, 256])
                wk = self.wload(w_in[:, :, 1024 + h * 256:1024 + h * 256 + 256], [128, KD, 256])
                if h == 0 and NG == 1:
                    qkb = [P.bank(0), P.bank(1), P.bank(2), P.bank(4)]
                    self.kouter([(qkb[i],
                                  (lambda k, i=i: (wq if i < 2 else wk).ap[:, k, (i % 2) * 128:(i % 2 + 1) * 128]),
                                  (lambda k: self.xop[k][g].ap),
                                  (lambda k, i=i: [wq if i < 2 else wk, self.xop[k][g]])) for i in range(4)])
                else:
                    qkb = [P.bank(pbank()) for _ in range(3)] + [P.bank(self.next_y_bank())]
                    for i in range(4):
                        wb = wq if i < 2 else wk
                        for k in range(KD):
                            self.mm(qkb[i], wb.ap[:, k, (i % 2) * 128:(i % 2 + 1) * 128], self.xop[k][g].ap, k == 0,
                                    k == KD - 1, reads=[wb, self.xop[k][g]])
                for which in range(2):
                    b1, b2 = qkb[2 * which], qkb[2 * which + 1]
                    cs, sn = (cos_t, sin_t) if which == 0 else (cos16, sin16)
                    dst = qT if which == 0 else kT
                    self.tt(ta, b1, cs, ALU.mult)
                    self.tt(tc, b1, sn, ALU.mult)
                    self.tt(tb, b2, sn, ALU.mult)
                    self.tt(td, b2, cs, ALU.mult)
                    self.tt(dst[0], ta, tb, ALU.subtract)
                    self.tt(dst[1], tc, td, ALU.add)
                    if which == 0:
                        for half in range(2):
                            o3 = qd[half].ap.rearrange("p (c t) -> p c t", t=128)
                            i3 = qT[half].ap.rearrange("p (c t) -> p c t", t=128)
                            P.op(P.dve, lambda e, o3=o3, i3=i3, qdec_b=qdec_b: e.tensor_tensor(o3, i3, qdec_b, ALU.mult),
                                 reads=[qT[half], self.b_qdec], writes=[qd[half]])

            def do_vkt(h):
                wb = self.wload(w_in[:, :, 2048 + h * 512:2048 + (h + 1) * 512], [128, KD, 512])
                for c in range(4):
                    bk = P.bank(pbank())
                    for k in range(KD):
                        self.mm(bk, self.xop[k][g].ap[:, c * 128:(c + 1) * 128], wb.ap[:, k, :], k == 0, k == KD - 1,
                                reads=[wb, self.xop[k][g]])
                    self.act_fn(vh[c], bk, AF.Copy)
                for c in range(4):
                    b = pbank()
                    for half in range(2):
                        o = P.bank(b, half * 128, (half + 1) * 128)
                        self.mm(o, kT[half].ap[:, c * 128:(c + 1) * 128], self.ident_bf.ap, True, True,
                                reads=[kT[half], self.ident_bf], signal=(half == 1))
                    bk = P.bank(b, 0, 256)
                    self.act_fn(kd[c], bk, AF.Identity, scale=self.b_kdec.ap[:, h:h + 1], extra_reads=[self.b_kdec])

            def do_chunks(h):
                nonlocal sci
                wgb = self.wload(w_in[:, :, 4096 + h * 512:4096 + (h + 1) * 512], [128, KD, 512])
                yh3 = yh.ap.rearrange("p (v t) -> p v t", t=512)
                for c in range(4):
                    cs_ = slice(c * 128, (c + 1) * 128)
                    bs = P.bank(3, 0, 128)
                    for half in range(2):
                        self.mm(bs, kT[half].ap[:, cs_], qT[half].ap[:, cs_], half == 0, half == 1,
                                reads=[kT[half], qT[half]])
                    sct = sc[sci % 2]
                    sci += 1
                    dts = P.sub(self.b_DT, h * 128, (h + 1) * 128, F32)
                    self.tt(sct, bs, dts, ALU.mult)
                    bk = P.bank(pbank())
                    for k in range(KD):
                        self.mm(bk, wgb.ap[:, k, c * 128:(c + 1) * 128], self.xop[k][g].ap, k == 0, k == KD - 1,
                                reads=[wgb, self.xop[k][g]])
                    self.act_fn(sgh[c], bk, AF.Silu)
                    by_b = self.next_y_bank()
                    for vc in range(4):
                        o = P.bank(by_b, vc * 128, (vc + 1) * 128)
                        self.mm(o, vh[c].ap[:, vc * 128:(vc + 1) * 128], sct.ap, True, False, reads=[vh[c], sct], signal=False)
                        for half in range(2):
                            self.mm(o, self.Sbf[h][half].ap[:, vc * 128:(vc + 1) * 128], qd[half].ap[:, cs_], False, half == 1,
                                    reads=[self.Sbf[h][half], qd[half]], signal=(half == 1 and vc == 3))
                    by = P.bank(by_b)
                    o_ap = yh3[:, :, cs_]
                    i_ap = by.ap.rearrange("p (v t) -> p v t", t=128)
                    P.op(P.act, lambda e, o_ap=o_ap, i_ap=i_ap: e.activation(o_ap, i_ap, AF.Copy), reads=[by], writes=[yh])
                    for half in range(2):
                        bS = P.bank(6 + half)
                        self.mm(bS, kd[c].ap[:, half * 128:(half + 1) * 128], vh[c].ap, True, True, reads=[kd[c], vh[c]])
                        S, Sb = self.S[h][half], self.Sbf[h][half]
                        self.stt(S, S, GAMMAS[h] ** 128, bS, ALU.mult, ALU.add)
                        self.act_fn(Sb, S, AF.Copy)

            def do_gn(h):
                yv = [P.sub(yh, vc * 512, (vc + 1) * 512, F32) for vc in range(4)]
                for vc in range(4):
                    self.act_fn(yb[vc], yv[vc], AF.Copy)
                    self.act_fn(ysq[vc], yv[vc], AF.Square)
                s1, s2 = P.bank(6), P.bank(7)
                for vc in range(4):
                    self.mm(s1, self.ones_bf.ap, yb[vc].ap, vc == 0, vc == 3, reads=[self.ones_bf, yb[vc]])
                for vc in range(4):
                    self.mm(s2, self.ones_bf.ap, ysq[vc].ap, vc == 0, vc == 3, reads=[self.ones_bf, ysq[vc]])
                m, v, r = self.m_t, self.v_t, self.r_t
                self.ts(m, s1, 1.0 / 512, None, ALU.mult)
                self.tt(v, m, m, ALU.mult)
                self.stt(v, s2, 1.0 / 512, v, ALU.mult, ALU.subtract)
                self.act_fn(r, v, AF.Ln, bias=self.const_ap(GN_EPS), extra_reads=[self.cst])
                self.act_fn(r, r, AF.Exp, scale=-0.5)
                for vc in range(4):
                    self.tt(yv[vc], yv[vc], m, ALU.subtract)
                    self.tt(yv[vc], yv[vc], r, ALU.mult)
                    self.tt(gated[h * 4 + vc][g], yv[vc], sgh[vc], ALU.mult)

            do_qk(0)
            self.flush_pending()
            for h in range(4):
                do_vkt(h)
                do_chunks(h)
                if h < 3:
                    do_qk(h + 1)
                do_gn(h)
        wo_slots = {}

        def yfn(dc, g, bk):
            if g == 0 or dc not in wo_slots:
                wo_slots[dc] = self.wload(w_out[:, :, dc * 128:(dc + 1) * 128], [128, 16, 128])
            sd = wo_slots[dc]
            for f in range(16):
                self.mm(bk, sd.ap[:, f, :], gated[f][g].ap, f == 0, f == 15, reads=[sd, gated[f][g]])

        self.resid_ln(lnidx, ALPHA, LN_EPS, yfn, zoff=z_off)
        P.sp_off = mark


def _build(nsub=6, ntiles=None, T=512):
    k = Kern(nsub=nsub, ntiles=ntiles, T=T)
    return k


_CACHE = {}


def kernel(**inputs):
    x = np.ascontiguousarray(inputs["x"], dtype=np.float32)
    if "nc" not in _CACHE:
        k = Kern()
        _CACHE["nc"] = k.build()
    nc = _CACHE["nc"]
    names = ["ln_g", "ln_b", "ffn_w_gate", "ffn_w_up", "ffn_w_down", "a_w_in", "a_ln_g", "a_ln_b",
             "a_w_s", "a_b_s", "a_w_out", "b_w_in", "b_w_out"]
    shared = {n: np.ascontiguousarray(inputs[n], dtype=np.float32) for n in names}
    in_maps = []
    for b in range(NB):
        m = dict(shared)
        m["x"] = x[b]
        in_maps.append(m)
    res = run_bass_kernel_spmd(nc, in_maps, core_ids=list(range(NB)))
    return np.stack([r["out"] for r in res.results], axis=0)
```

```python
import math
from contextlib import ExitStack

import numpy as np
import concourse.bass as bass
import concourse.mybir as mybir
from concourse.bass_utils import run_bass_kernel_spmd

F32 = mybir.dt.float32
BF16 = mybir.dt.bfloat16
I32 = mybir.dt.int32
AF = mybir.ActivationFunctionType
ALU = mybir.AluOpType
AX = mybir.AxisListType

D = 1024
SEQ = 4096
NB = 8
DFF = 2816
KD = D // 128
KF = DFF // 128
DEPTH = 2
ALPHA = float((2 * DEPTH) ** 0.25)
LN_EPS = 1e-5
GN_EPS = 1e-6
CELL = 256
SLOT_BYTES = 8192
NSLOT = 4
GAMMAS = [1.0 - 2.0 ** (-5.0 - h) for h in range(4)]
LOG_GAMMAS = [math.log1p(-(2.0 ** (-5.0 - h))) for h in range(4)]


class Tl:
    def __init__(self, ap, space, c0, c1):
        self.ap = ap
        self.space = space
        self.c0 = c0
        self.c1 = c1

    def cells(self):
        return [(self.space, c) for c in range(self.c0, self.c1)]


class Eng:
    def __init__(self, prog, name, is_pe=False):
        self.prog = prog
        self.name = name
        self.is_pe = is_pe
        self.q = []
        self.sem = prog.new_sem(name)
        self.cnt = 0
        self.known = {}


class Prog:
    def __init__(self, nc, stack):
        self.nc = nc
        self.stack = stack
        self.sems = []
        self.cells = {}
        self.pe_sems = set()
        self.pe = Eng(self, "pe", True)
        self.pe_sems.add(self.pe.sem)
        self.act = Eng(self, "act")
        self.dve = Eng(self, "dve")
        self.pool = Eng(self, "pool")
        self.sp = Eng(self, "sp")
        self.dma_cnt = {}
        self.arena_bytes = 206 * 1024
        self.arena = stack.enter_context(nc.sbuf_tensor("arena", [128, self.arena_bytes // 4], F32))
        self.sp_off = 0
        self.psum = stack.enter_context(nc.psum_tensor("ps", [128, 8, 512], F32))

    def new_sem(self, name):
        h = self.stack.enter_context(self.nc.semaphore("s%d_%s" % (len(self.sems), name)))
        self.sems.append(h)
        return len(self.sems) - 1

    def alloc(self, nbytes):
        nbytes = (nbytes + CELL - 1) // CELL * CELL
        off = self.sp_off
        self.sp_off += nbytes
        self.hwm = max(getattr(self, "hwm", 0), self.sp_off)
        assert self.sp_off <= self.arena_bytes, "arena overflow %d" % self.sp_off
        return off

    def view(self, off, nelem, dt):
        esz = 4 if dt in (F32, I32) else 2
        nb = nelem * esz
        assert off % 4 == 0 and nb % 4 == 0
        ap = self.arena[:, off // 4:(off + nb) // 4]
        if dt != F32:
            ap = ap.bitcast(dt)
        return Tl(ap, "sb", off // CELL, (off + nb + CELL - 1) // CELL)

    def tile(self, nelem, dt):
        esz = 4 if dt in (F32, I32) else 2
        return self.view(self.alloc(nelem * esz), nelem, dt)

    def sub(self, tl, lo, hi, dt):
        esz = 4 if dt in (F32, I32) else 2
        base = tl.c0 * CELL
        return Tl(tl.ap[:, lo:hi], "sb", (base + lo * esz) // CELL, (base + hi * esz + CELL - 1) // CELL)

    def bank(self, b, lo=0, hi=512):
        return Tl(self.psum[:, b, lo:hi], "ps", b, b + 1)

    def op(self, eng, fn, reads=(), writes=(), signal=True, dma_sem=None):
        deps = {}
        cells = self.cells
        ps_reads = [t for t in reads if t.space == "ps"]
        if ps_reads:
            reads = [t for t in reads if t.space != "ps"]
            writes = list(writes) + ps_reads
        for t in reads:
            for c in t.cells():
                st = cells.get(c)
                if st is not None and st[0] is not None:
                    s, v = st[0]
                    if deps.get(s, 0) < v:
                        deps[s] = v
        for t in writes:
            for c in t.cells():
                st = cells.get(c)
                if st is not None:
                    if st[0] is not None:
                        s, v = st[0]
                        if deps.get(s, 0) < v:
                            deps[s] = v
                    for s, v in st[1].items():
                        if deps.get(s, 0) < v:
                            deps[s] = v
        waits = []
        for s, v in deps.items():
            if eng.is_pe and s in self.pe_sems:
                continue
            if eng.known.get(s, 0) >= v:
                continue
            eng.known[s] = v
            waits.append((s, v))
        if dma_sem is not None:
            self.dma_cnt[dma_sem] = self.dma_cnt.get(dma_sem, 0) + 16
            evt = (dma_sem, self.dma_cnt[dma_sem])
            inc = (dma_sem, 16)
        else:
            evt = (eng.sem, eng.cnt + 1)
            if signal:
                eng.cnt += 1
                inc = (eng.sem, 1)
            else:
                inc = None
        for t in reads:
            for c in t.cells():
                st = cells.get(c)
                if st is None:
                    st = cells[c] = [None, {}]
                if st[1].get(evt[0], 0) < evt[1]:
                    st[1][evt[0]] = evt[1]
        for t in writes:
            for c in t.cells():
                cells[c] = [evt, {}]
        eng.q.append((waits, fn, inc))
        return evt

    def emit(self):
        nc = self.nc
        sems = self.sems

        def run(q):
            def body(e):
                for waits, fn, inc in q:
                    for s, v in waits:
                        e.wait_ge(sems[s], v)
                    ins = fn(e)
                    if inc is not None:
                        ins.then_inc(sems[inc[0]], inc[1])
            return body

        with nc.Block() as block:
            block.tensor(run(self.pe.q))
            block.scalar(run(self.act.q))
            block.vector(run(self.dve.q))
            block.gpsimd(run(self.pool.q))
            block.sync(run(self.sp.q))


class Kern:
    def __init__(self, nsub=6, ntiles=None, T=512):
        self.T = T
        self.NG = T // 512
        self.ntiles = ntiles if ntiles is not None else SEQ // T
        self.nsub = nsub
        self.nc = nc = bass.Bass("TRN2", target_bir_lowering=False)
        dt = nc.dram_tensor
        self.x = dt("x", [SEQ, D], F32, kind="ExternalInput").ap()
        self.ln_g = dt("ln_g", [2, 3, D], F32, kind="ExternalInput").ap()
        self.ln_b = dt("ln_b", [2, 3, D], F32, kind="ExternalInput").ap()
        self.wg = dt("ffn_w_gate", [2, 2, D, DFF], F32, kind="ExternalInput").ap()
        self.wu = dt("ffn_w_up", [2, 2, D, DFF], F32, kind="ExternalInput").ap()
        self.wd = dt("ffn_w_down", [2, 2, DFF, D], F32, kind="ExternalInput").ap()
        self.a_w_in = dt("a_w_in", [1, D, 4096], F32, kind="ExternalInput").ap()
        self.a_ln_g = dt("a_ln_g", [1, 2048], F32, kind="ExternalInput").ap()
        self.a_ln_b = dt("a_ln_b", [1, 2048], F32, kind="ExternalInput").ap()
        self.a_w_s = dt("a_w_s", [1, 8, 128, 128], F32, kind="ExternalInput").ap()
        self.a_b_s = dt("a_b_s", [1, 8, 128], F32, kind="ExternalInput").ap()
        self.a_w_out = dt("a_w_out", [1, 2048, D], F32, kind="ExternalInput").ap()
        self.b_w_in = dt("b_w_in", [1, D, 6144], F32, kind="ExternalInput").ap()
        self.b_w_out = dt("b_w_out", [1, 2048, D], F32, kind="ExternalInput").ap()
        self.out = dt("out", [SEQ, D], F32, kind="ExternalOutput").ap()

    def mm(self, out, lhsT, rhs, start, stop, reads, signal=None):
        P = self.P
        if signal is None:
            signal = stop
        o, l, r = out.ap, lhsT, rhs
        P.op(P.pe, lambda e: e.matmul(o, l, r, start=start, stop=stop), reads=reads, writes=[out], signal=signal)

    def act_fn(self, out, in_, func, reads=None, bias=None, scale=None, extra_reads=(), accum=None):
        P = self.P
        kw = {}
        if bias is not None:
            kw["bias"] = bias
        if scale is not None:
            kw["scale"] = scale
        wr = [out]
        if accum is not None:
            kw["accum_out"] = accum.ap
            wr.append(accum)
        o, i = out.ap, in_.ap
        P.op(P.act, lambda e: e.activation(o, i, func, **kw), reads=[in_] + list(extra_reads), writes=wr)

    def tt(self, out, in0, in1, op, eng=None):
        P = self.P
        eng = eng or P.dve
        o, a, b = out.ap, in0.ap, in1.ap
        P.op(eng, lambda e: e.tensor_tensor(o, a, b, op), reads=[in0, in1], writes=[out])

    def ts(self, out, in0, s1, s2, op0, op1=None, extra_reads=(), eng=None, accum=None):
        P = self.P
        eng = eng or P.dve
        o, a = out.ap, in0.ap
        wr = [out]
        kw = {}
        if accum is not None:
            kw["accum_out"] = accum.ap
            wr.append(accum)
        if op1 is None:
            P.op(eng, lambda e: e.tensor_scalar(o, a, s1, None, op0, **kw), reads=[in0] + list(extra_reads), writes=wr)
        else:
            P.op(eng, lambda e: e.tensor_scalar(o, a, s1, s2, op0, op1, **kw), reads=[in0] + list(extra_reads), writes=wr)

    def stt(self, out, in0, scalar, in1, op0, op1, extra_reads=(), accum=None):
        P = self.P
        o, a, b = out.ap, in0.ap, in1.ap
        wr = [out]
        kw = {}
        if accum is not None:
            kw["accum_out"] = accum.ap
            wr.append(accum)
        P.op(P.dve, lambda e: e.scalar_tensor_tensor(o, a, scalar, b, op0, op1, **kw),
             reads=[in0, in1] + list(extra_reads), writes=wr)

    def wload(self, src_ap, shape):
        P = self.P
        s = self.slot_i % NSLOT
        self.slot_i += 1
        n = 1
        for d in shape[1:]:
            n *= d
        assert n * 2 <= SLOT_BYTES
        full = self.slots[s]
        v = full.ap[:, 0:n]
        if len(shape) == 3:
            v = v.rearrange("p (a b) -> p a b", b=shape[2])
        tl = Tl(v, "sb", full.c0, full.c1)
        P.op(P.pool, lambda e: e.dma_start(out=v, in_=src_ap), reads=[], writes=[tl], dma_sem=self.slot_sems[s])
        return tl

    def build(self):
        nc = self.nc
        with ExitStack() as stack:
            self.P = P = Prog(nc, stack)
            T, NG = self.T, self.NG
            self.slots = [P.tile(SLOT_BYTES // 2, BF16) for _ in range(NSLOT)]
            self.slot_sems = [P.new_sem("slot%d" % i) for i in range(NSLOT)]
            self.slot_i = 0
            self.in_sems = [P.new_sem("in%d" % i) for i in range(4)]
            self.out_sems = [P.new_sem("out%d" % i) for i in range(4)]
            self.c_sem = P.new_sem("const")
            self.xres = [[P.tile(512, F32) for g in range(NG)] for dc in range(KD)]
            self.xop = [[P.tile(512, BF16) for g in range(NG)] for dc in range(KD)]
            self.ident = P.tile(128, F32)
            self.ones_bf = P.tile(128, BF16)
            self.cst = P.tile(64, F32)
            self.dummy = P.tile(64, F32)
            self.next_func = None
            self.pending = []
            self.bg_ops = []
            self.last_sub = False
            self.cst_cols = {}
            self.lngb = P.tile(12 * KD, F32)
            self.lng = P.sub(self.lngb, 0, 6 * KD, F32)
            self.lnb = P.sub(self.lngb, 6 * KD, 12 * KD, F32)
            self.m_t = P.tile(512, F32)
            self.v_t = P.tile(512, F32)
            self.r_t = P.tile(512, F32)
            self.sg_t = [P.tile(512, F32) for _ in range(2)]
            self.sg_i = 0
            self.scratch_base = P.sp_off
            self.mm_banks = [0, 1, 2, 3]
            self.mm_i = 0
            self.y_banks = [4, 5]
            self.y_i = 0

            self.setup_consts()
            if self.nsub >= 2:
                self.setup_mixer_a()
            if self.nsub >= 5:
                self.setup_mixer_b()
            self.scratch_base = P.sp_off
            self.load_dma(0)
            for ti in range(self.ntiles):
                self.load_tile(ti)
                si = 0
                for li in range(DEPTH):
                    for part in range(3):
                        if si >= self.nsub:
                            break
                        last = (si == self.nsub - 1)
                        if last and ti + 1 < self.ntiles and part != 1:
                            self.load_dma(ti + 1)
                        self.next_func = AF.Gelu if (li == 0 and part == 0) else AF.Silu
                        self.last_sub = last
                        if part == 0:
                            if li == 1 and ti > 0 and self.nsub >= 5:
                                self.rotary_tables(ti, background=True)
                            self.ffn(li, 0, li * 3 + 0)
                        elif part == 1:
                            if li == 0:
                                if ti == 0:
                                    self.emit_mixer_a()
                                self.mixer_a(li * 3 + 1, ti)
                            else:
                                if ti == 0:
                                    self.emit_mixer_b()
                                    self.rotary_tables(0)
                                self.mixer_b(li * 3 + 1, ti)
                        else:
                            self.ffn(li, 1, li * 3 + 2)
                        si += 1
                if ti + 1 < self.ntiles and (self.nsub == 0 or (self.nsub - 1) % 3 == 1):
                    self.load_dma(ti + 1)
                self.store_tile(ti)
            P.sp.q.append(([(s_, P.dma_cnt.get(s_, 0)) for s_ in self.out_sems], lambda e: e.nop(), None))
            P.emit()
        return nc

    def kouter(self, accs):
        for k in range(KD):
            for (bk, lf, rf, rdf) in accs:
                self.mm(bk, lf(k), rf(k), k == 0, k == KD - 1, reads=rdf(k))

    def next_mm_bank(self):
        b = self.mm_banks[self.mm_i % len(self.mm_banks)]
        self.mm_i += 1
        return b

    def next_y_bank(self):
        b = self.y_banks[self.y_i % len(self.y_banks)]
        self.y_i += 1
        return b

    def setup_consts(self):
        P = self.P
        nc = self.nc
        idt = self.ident
        P.op(P.pool, lambda e: e.memset(idt.ap, 1.0), writes=[idt])
        P.op(P.pool, lambda e: e.affine_select(idt.ap, idt.ap, [[1, 128]], ALU.is_equal, 0.0, base=0,
                                               channel_multiplier=-1), reads=[idt], writes=[idt])
        ob = self.ones_bf
        P.op(P.pool, lambda e: e.memset(ob.ap, 1.0), writes=[ob])
        for l in range(2):
            for j in range(3):
                idx = (l * 3 + j) * KD
                for (dst, src) in ((self.lng, self.ln_g), (self.lnb, self.ln_b)):
                    d_ap = dst.ap[:, idx:idx + KD]
                    s_ap = src[l, j].rearrange("(c p) -> p c", p=128)
                    P.op(P.sp, lambda e, d_ap=d_ap, s_ap=s_ap: e.dma_start(out=d_ap, in_=s_ap,
                                                                          allow_slow_non_contiguous=True),
                         writes=[dst], dma_sem=self.c_sem)

    def load_dma(self, ti):
        P = self.P
        self.in_st = []
        for g in range(self.NG):
            st = [P.view(self.scratch_base + 40960 + (g * 4 + c) * 4096, D, F32) for c in range(4)]
            self.in_st.append(st)
            for c in range(4):
                r0 = ti * self.T + g * 512 + c * 128
                src = self.x[r0:r0 + 128, :]
                d_ap = st[c].ap
                P.op(P.sp, lambda e, d_ap=d_ap, src=src: e.dma_start(out=d_ap, in_=src), writes=[st[c]],
                     dma_sem=self.in_sems[c])

    def load_tile(self, ti):
        P = self.P
        for g in range(self.NG):
            st = self.in_st[g]
            for dc in range(KD):
                b = self.next_mm_bank()
                for c in range(4):
                    o = P.bank(b, c * 128, (c + 1) * 128)
                    i_ap = st[c].ap[:, dc * 128:(dc + 1) * 128]
                    idt = self.ident
                    P.op(P.pe, lambda e, o=o, i_ap=i_ap, idt=idt: e.transpose(o.ap, i_ap, idt.ap),
                         reads=[st[c], idt], writes=[o], signal=(c == 3))
                bk = P.bank(b)
                xr, xo = self.xres[dc][g], self.xop[dc][g]
                P.op(P.act, lambda e, xr=xr, bk=bk: e.activation(xr.ap, bk.ap, AF.Copy), reads=[bk], writes=[xr])
                P.op(P.dve, lambda e, xo=xo, xr=xr: e.tensor_copy(xo.ap, xr.ap), reads=[xr], writes=[xo])

    def store_tile(self, ti):
        P = self.P
        self.flush_pending()
        mark = P.sp_off
        for g in range(self.NG):
            st = [P.tile(D, F32) for _ in range(4)]
            for c in range(4):
                for half in range(2):
                    b = self.next_mm_bank()
                    for q in range(4):
                        dc = half * 4 + q
                        o = P.bank(b, q * 128, (q + 1) * 128)
                        xr = self.xres[dc][g]
                        i_ap = xr.ap[:, c * 128:(c + 1) * 128]
                        idt = self.ident
                        P.op(P.pe, lambda e, o=o, i_ap=i_ap, idt=idt: e.transpose(o.ap, i_ap, idt.ap),
                             reads=[xr, idt], writes=[o], signal=(q == 3))
                    bk = P.bank(b)
                    dst = P.sub(st[c], half * 512, (half + 1) * 512, F32)
                    if half == 0:
                        P.op(P.act, lambda e, dst=dst, bk=bk: e.activation(dst.ap, bk.ap, AF.Copy), reads=[bk], writes=[dst])
                    else:
                        P.op(P.dve, lambda e, dst=dst, bk=bk: e.tensor_copy(dst.ap, bk.ap), reads=[bk], writes=[dst])
                r0 = ti * self.T + g * 512 + c * 128
                dst_ap = self.out[r0:r0 + 128, :]
                s_ap = st[c].ap
                P.op(P.sp, lambda e, dst_ap=dst_ap, s_ap=s_ap: e.dma_start(out=dst_ap, in_=s_ap), reads=[st[c]],
                     dma_sem=self.out_sems[c])
        P.sp_off = mark

    def ln_stats_mm(self, g, dc, s1, s2):
        self.mm(s1, self.ones_bf.ap, self.zb[dc].ap, dc == 0, dc == KD - 1, reads=[self.ones_bf, self.zb[dc]])
        self.mm(s2, self.ones_bf.ap, self.zsq[dc].ap, dc == 0, dc == KD - 1, reads=[self.ones_bf, self.zsq[dc]])

    def ln_finalize(self, g, lnidx, eps, s1, s2):
        P = self.P
        m, v, r = self.m_t, self.v_t, self.r_t
        self.ts(m, s1, 1.0 / D, None, ALU.mult)
        self.tt(v, m, m, ALU.mult)
        self.stt(v, s2, 1.0 / D, v, ALU.mult, ALU.subtract)
        self.act_fn(r, v, AF.Ln, bias=self.const_ap(eps), extra_reads=[self.cst])
        self.act_fn(r, r, AF.Exp, scale=-0.5)
        gb = []
        for dc in range(KD):
            z = self.xres[dc][g]
            self.tt(z, z, m, ALU.subtract)
            self.tt(z, z, r, ALU.mult)
            gi = lnidx * KD + dc
            g_ap = self.lng.ap[:, gi:gi + 1]
            b_ap = self.lnb.ap[:, gi:gi + 1]
            gb.append((g_ap, b_ap))
            if self.last_sub:
                self.act_fn(z, z, AF.Identity, bias=b_ap, scale=g_ap, extra_reads=[self.lng, self.lnb])
                continue
            self.act_fn(self.xop[dc][g], z, AF.Identity, bias=b_ap, scale=g_ap, extra_reads=[self.lng, self.lnb])
        if self.last_sub:
            return
        if self.next_func is not None:
            self.act_fn(P.sub(self.dummy, 0, 1, F32), self.const_tl(1.0), self.next_func)

        def finish(dc_list=tuple(range(KD)), g=g, gb=gb):
            for dc in dc_list:
                z = self.xres[dc][g]
                g_ap, b_ap = gb[dc]
                self.act_fn(z, z, AF.Identity, bias=b_ap, scale=g_ap, extra_reads=[self.lng, self.lnb])

        self.pending.append(finish)

    def flush_pending(self):
        pend, self.pending = self.pending, []
        for f in pend:
            f()

    def const_tl(self, val):
        self.const_ap(val)
        i = self.cst_cols[val]
        return self.P.sub(self.cst, i, i + 1, F32)

    def const_ap(self, val):
        P = self.P
        if val not in self.cst_cols:
            i = len(self.cst_cols)
            assert i < 16
            self.cst_cols[val] = i
            col = self.cst.ap[:, i:i + 1]
            P.op(P.pool, lambda e: e.memset(col, float(val)), writes=[self.cst])
        i = self.cst_cols[val]
        return self.cst.ap[:, i:i + 1]

    def resid_ln(self, lnidx, c_res, eps, ybank_fn, zoff=None):
        P = self.P
        self.flush_pending()
        if zoff is None:
            self.zb = [P.tile(512, BF16) for _ in range(KD)]
            self.zsq = [P.tile(512, BF16) for _ in range(KD)]
        else:
            self.zb = [P.view(zoff + i * 1024, 512, BF16) for i in range(KD)]
            self.zsq = [P.view(zoff + 8192 + i * 1024, 512, BF16) for i in range(KD)]
        for g in range(self.NG):
            s1, s2 = P.bank(6), P.bank(7)
            prev = None
            self.act_fn(P.sub(self.dummy, 0, 1, F32), self.const_tl(1.0), AF.Ln, bias=self.const_ap(1.0), extra_reads=[self.cst])
            for dc in range(KD):
                bk = P.bank(self.next_y_bank())
                ybank_fn(dc, g, bk)
                if prev is not None:
                    self.ln_stats_mm(g, prev, s1, s2)
                z = self.xres[dc][g]
                self.stt(z, z, c_res, bk, ALU.mult, ALU.add)
                self.act_fn(self.zb[dc], z, AF.Copy)
                self.act_fn(self.zsq[dc], z, AF.Square)
                prev = dc
            self.ln_stats_mm(g, prev, s1, s2)
            self.ln_finalize(g, lnidx, eps, s1, s2)

    def ffn(self, li, j, lnidx):
        P = self.P
        NG = self.NG
        mark = P.sp_off
        h = [[P.tile(512, BF16) for g in range(NG)] for f in range(KF)]
        wg = self.wg[li, j].rearrange("(k p) f -> p k f", p=128)
        wu = self.wu[li, j].rearrange("(k p) f -> p k f", p=128)
        wd = self.wd[li, j].rearrange("(f p) d -> p f d", p=128)
        first = True
        for c0 in range(0, DFF, 512):
            n = min(512, DFF - c0)
            sg = self.wload(wg[:, :, c0:c0 + n], [128, KD, n])
            su = self.wload(wu[:, :, c0:c0 + n], [128, KD, n])
            fls = list(range(n // 128))
            if first and NG == 1:
                first = False
                g = 0
                accs = []
                for fl in fls[:3]:
                    for (w, b) in ((sg, 2 * fl), (su, 2 * fl + 1)):
                        accs.append((P.bank(b),
                                     (lambda k, w=w, fl=fl: w.ap[:, k, fl * 128:(fl + 1) * 128]),
                                     (lambda k: self.xop[k][g].ap),
                                     (lambda k, w=w: [w, self.xop[k][g]])))
                self.kouter(accs)
                for fl in fls[:3]:
                    tmp = self.sg_t[self.sg_i % 2]
                    self.sg_i += 1
                    self.act_fn(tmp, P.bank(2 * fl), AF.Silu)
                    self.tt(h[c0 // 128 + fl][g], tmp, P.bank(2 * fl + 1), ALU.mult)
                self.flush_pending()
                fls = fls[3:]
            for fl in fls:
                fc = c0 // 128 + fl
                for g in range(NG):
                    bg = P.bank(self.next_mm_bank())
                    bu = P.bank(self.next_mm_bank())
                    for k in range(KD):
                        self.mm(bg, sg.ap[:, k, fl * 128:(fl + 1) * 128], self.xop[k][g].ap, k == 0, k == KD - 1,
                                reads=[sg, self.xop[k][g]])
                    for k in range(KD):
                        self.mm(bu, su.ap[:, k, fl * 128:(fl + 1) * 128], self.xop[k][g].ap, k == 0, k == KD - 1,
                                reads=[su, self.xop[k][g]])
                    tmp = self.sg_t[self.sg_i % 2]
                    self.sg_i += 1
                    self.act_fn(tmp, bg, AF.Silu)
                    self.tt(h[fc][g], tmp, bu, ALU.mult)
                    self.bg_step(2)

        wd_slots = {}

        def yfn(dc, g, bk):
            if g == 0 or dc not in wd_slots:
                wd_slots[dc] = self.wload(wd[:, :, dc * 128:(dc + 1) * 128], [128, KF, 128])
            sd = wd_slots[dc]
            for f in range(KF):
                self.mm(bk, sd.ap[:, f, :], h[f][g].ap, f == 0, f == KF - 1, reads=[sd, h[f][g]])

        self.resid_ln(lnidx, 2.0 * ALPHA, 4.0 * LN_EPS, yfn)
        P.sp_off = mark

    def setup_mixer_a(self):
        P = self.P
        self.a_wsT = P.tile(8 * 128, BF16)
        self.a_blhs = P.tile(2048, BF16)
        self.a_brhs = P.tile(1024, BF16)
        self.a_G = P.tile(2048, F32)
        self.a_sem = P.new_sem("a_const")

    def emit_mixer_a(self):
        P = self.P
        mark = P.sp_off
        wst = P.tile(1024, F32)
        wT = P.tile(1024, F32)
        bs_f = P.tile(1024, F32)
        bs_h = P.tile(1024, BF16)
        bs_l = P.tile(1024, BF16)
        ones_f = P.tile(1, F32)
        csem = self.a_sem
        src = self.a_w_s[0].rearrange("g t s -> t g s")
        d3 = wst.ap.rearrange("p (g s) -> p g s", s=128)
        P.op(P.sp, lambda e: e.dma_start(out=d3, in_=src), writes=[wst], dma_sem=P.new_sem("ac"))
        P.op(P.pool, lambda e: e.memset(ones_f.ap, 1.0), writes=[ones_f])
        for half in range(2):
            b = self.next_mm_bank()
            for q in range(4):
                g = half * 4 + q
                o = P.bank(b, q * 128, (q + 1) * 128)
                i_ap = wst.ap[:, g * 128:(g + 1) * 128]
                idt = self.ident
                P.op(P.pe, lambda e, o=o, i_ap=i_ap, idt=idt: e.transpose(o.ap, i_ap, idt.ap), reads=[wst, idt],
                     writes=[o], signal=(q == 3))
            bk = P.bank(b)
            dst = P.sub(wT, half * 512, (half + 1) * 512, F32)
            P.op(P.act, lambda e, dst=dst, bk=bk: e.activation(dst.ap, bk.ap, AF.Copy), reads=[bk], writes=[dst])
        P.op(P.pool, lambda e: e.affine_select(wT.ap, wT.ap, [[0, 8], [1, 128]], ALU.is_ge, 0.0, base=0,
                                               channel_multiplier=-1), reads=[wT], writes=[wT])
        wsT = self.a_wsT
        P.op(P.dve, lambda e: e.tensor_copy(wsT.ap, wT.ap), reads=[wT], writes=[wsT])
        brhs = self.a_brhs
        for half in range(2):
            bk = P.bank(self.next_mm_bank())
            o_ap = bk.ap[0:1, :]
            r_ap = wT.ap[:, half * 512:(half + 1) * 512]
            l_ap = ones_f.ap
            P.op(P.pe, lambda e, o_ap=o_ap, l_ap=l_ap, r_ap=r_ap: e.matmul(o_ap, l_ap, r_ap, start=True, stop=True),
                 reads=[ones_f, wT], writes=[bk])
            d_ap = brhs.ap[0:1, half * 512:(half + 1) * 512]
            P.op(P.act, lambda e, d_ap=d_ap, o_ap=o_ap: e.activation(d_ap, o_ap, AF.Copy), reads=[bk], writes=[brhs])
        bsrc = self.a_b_s[0:1].rearrange("o g t -> o (g t)")
        P.op(P.sp, lambda e: e.dma_start(out=bs_f.ap[0:1, :], in_=bsrc), writes=[bs_f], dma_sem=P.new_sem("ac"))
        P.op(P.dve, lambda e: e.tensor_copy(bs_h.ap[0:1, :], bs_f.ap[0:1, :]), reads=[bs_f], writes=[bs_h])
        P.op(P.dve, lambda e: e.tensor_tensor(bs_l.ap[0:1, :], bs_f.ap[0:1, :], bs_h.ap[0:1, :], ALU.subtract),
             reads=[bs_f, bs_h], writes=[bs_l])
        P.op(P.sp, lambda e: e.dma_start(out=brhs.ap[1:2, :], in_=bs_h.ap[0:1, :]), reads=[bs_h], writes=[brhs],
             dma_sem=P.new_sem("ac"))
        P.op(P.sp, lambda e: e.dma_start(out=brhs.ap[2:3, :], in_=bs_l.ap[0:1, :]), reads=[bs_l], writes=[brhs],
             dma_sem=P.new_sem("ac"))
        blhs = self.a_blhs
        P.op(P.pool, lambda e: e.memset(blhs.ap[0:3, :], 1.0), writes=[blhs])
        P.op(P.pool, lambda e: e.dma_start(out=blhs.ap[0:1, :], in_=self.a_ln_b[0:1, :]), writes=[blhs],
             dma_sem=P.new_sem("ac"))
        G = self.a_G
        gsrc = self.a_ln_g[0:1, :].partition_broadcast(128)
        g3 = G.ap.rearrange("p (o f) -> p o f", o=1)
        P.op(P.sp, lambda e: e.dma_start(out=g3, in_=gsrc), writes=[G], dma_sem=P.new_sem("ac"))
        P.sp_off = mark

    def mixer_a(self, lnidx, ti):
        P = self.P
        NG = self.NG
        mark = P.sp_off
        w_in = self.a_w_in[0].rearrange("(k p) f -> p k f", p=128)
        w_out = self.a_w_out[0].rearrange("(f p) d -> p f d", p=128)
        vt_off = P.sp_off
        vt = [[P.tile(2048, F32) for c in range(4)] for g in range(NG)]
        vln = [[P.tile(2048, BF16) for c in range(4)] for g in range(NG)]
        ut = [[(P.tile(512, F32) if f < 4 else P.view(vt_off + g * 32768 + (f - 4) * 2048, 512, F32))
               for f in range(16)] for g in range(NG)]
        gated = [[P.tile(512, BF16) for g in range(NG)] for f in range(16)]
        s1 = [P.tile(16, F32) for g in range(NG)]
        s2 = [P.tile(16, F32) for g in range(NG)]
        st4 = [P.tile(16, F32) for g in range(NG)]
        junk = [P.tile(512, F32) for _ in range(2)]
        for vg in range(4):
            wb = self.wload(w_in[:, :, 2048 + vg * 512:2048 + (vg + 1) * 512], [128, KD, 512])
            for g in range(NG):
                kout = (vg == 0 and NG == 1)
                if kout:
                    self.kouter([(P.bank(c),
                                  (lambda k, c=c: self.xop[k][g].ap[:, c * 128:(c + 1) * 128]),
                                  (lambda k, wb=wb: wb.ap[:, k, :]),
                                  (lambda k, wb=wb: [wb, self.xop[k][g]])) for c in range(4)])
                for c in range(4):
                    if kout:
                        bk = P.bank(c)
                    else:
                        bk = P.bank(self.next_mm_bank())
                        for k in range(KD):
                            self.mm(bk, self.xop[k][g].ap[:, c * 128:(c + 1) * 128], wb.ap[:, k, :], k == 0, k == KD - 1,
                                    reads=[wb, self.xop[k][g]])
                    vsl = P.sub(vt[g][c], vg * 512, (vg + 1) * 512, F32)
                    a1 = P.sub(s1[g], c * 4 + vg, c * 4 + vg + 1, F32)
                    self.act_fn(vsl, bk, AF.Gelu, accum=a1)
                    a2 = P.sub(s2[g], c * 4 + vg, c * 4 + vg + 1, F32)
                    jk = junk[(c + vg) % 2]
                    self.stt(jk, vsl, 1.0, vsl, ALU.mult, ALU.mult, accum=a2)
        self.flush_pending()
        for ug in range(4):
            wb = self.wload(w_in[:, :, ug * 512:(ug + 1) * 512], [128, KD, 512])
            for fl in range(4):
                fc = ug * 4 + fl
                for g in range(NG):
                    bk = P.bank(self.next_mm_bank())
                    for k in range(KD):
                        self.mm(bk, wb.ap[:, k, fl * 128:(fl + 1) * 128], self.xop[k][g].ap, k == 0, k == KD - 1,
                                reads=[wb, self.xop[k][g]])
                    self.act_fn(ut[g][fc], bk, AF.Gelu)
            if ug == 0:
                for g in range(NG):
                    st = st4[g]
                    mean = P.sub(st, 0, 4, F32)
                    e2 = P.sub(st, 4, 8, F32)
                    var = P.sub(st, 8, 12, F32)
                    tmp = P.sub(st, 12, 16, F32)
                    s1v = s1[g].ap.rearrange("p (c v) -> p c v", v=4)
                    s2v = s2[g].ap.rearrange("p (c v) -> p c v", v=4)
                    P.op(P.dve, lambda e, mean=mean, s1v=s1v: e.tensor_reduce(mean.ap, s1v, AX.X, ALU.add),
                         reads=[s1[g]], writes=[mean])
                    P.op(P.dve, lambda e, e2=e2, s2v=s2v: e.tensor_reduce(e2.ap, s2v, AX.X, ALU.add),
                         reads=[s2[g]], writes=[e2])
                    self.ts(mean, mean, 1.0 / 2048, None, ALU.mult)
                    self.tt(tmp, mean, mean, ALU.mult)
                    self.stt(var, e2, 1.0 / 2048, tmp, ALU.mult, ALU.subtract)
                    self.act_fn(var, var, AF.Sqrt, bias=self.const_ap(LN_EPS), extra_reads=[self.cst])
                    P.op(P.dve, lambda e, var=var: e.reciprocal(var.ap, var.ap), reads=[var], writes=[var])
                    for c in range(4):
                        m_ap = mean.ap[:, c:c + 1]
                        r_ap = var.ap[:, c:c + 1]
                        self.ts(vt[g][c], vt[g][c], m_ap, r_ap, ALU.subtract, ALU.mult, extra_reads=[st])
                        self.tt(vln[g][c], vt[g][c], self.a_G, ALU.mult)
        for fc in range(16):
            G8 = fc // 2
            for g in range(NG):
                bk = P.bank(self.next_mm_bank())
                for c in range(4):
                    o = P.bank(bk.c0, c * 128, (c + 1) * 128)
                    self.mm(o, vln[g][c].ap[:, fc * 128:(fc + 1) * 128], self.a_wsT.ap[:, G8 * 128:(G8 + 1) * 128],
                            True, False, reads=[vln[g][c], self.a_wsT], signal=False)
                    self.mm(o, self.a_blhs.ap[0:3, fc * 128:(fc + 1) * 128], self.a_brhs.ap[0:3, G8 * 128:(G8 + 1) * 128],
                            False, True, reads=[self.a_blhs, self.a_brhs], signal=(c == 3))
                self.tt(gated[fc][g], ut[g][fc], bk, ALU.mult)
        wo_slots = {}

        def yfn(dc, g, bk):
            if g == 0 or dc not in wo_slots:
                wo_slots[dc] = self.wload(w_out[:, :, dc * 128:(dc + 1) * 128], [128, 16, 128])
            sd = wo_slots[dc]
            for f in range(16):
                self.mm(bk, sd.ap[:, f, :], gated[f][g].ap, f == 0, f == 15, reads=[sd, gated[f][g]])

        self.resid_ln(lnidx, ALPHA, LN_EPS, yfn, zoff=vt_off + 24576 if NG == 1 else None)
        P.sp_off = mark

    def setup_mixer_b(self):
        P = self.P
        self.b_DT = P.tile(512, F32)
        self.b_qdec = P.tile(512, F32)
        self.b_kdec = P.tile(4, F32)
        self.b_pos0 = P.tile(512, F32)
        self.b_inv = P.tile(1, F32)
        self.ident_bf = P.tile(128, BF16)
        self.rot_tabs = [P.tile(512, F32) for _ in range(4)]
        self.rot_tmp = [P.tile(512, F32) for _ in range(3)]
        self.S = [[P.tile(512, F32) for half in range(2)] for h in range(4)]
        self.Sbf = [[P.tile(512, BF16) for half in range(2)] for h in range(4)]

    def emit_mixer_b(self):
        P = self.P
        mark = P.sp_off
        rel = P.tile(128, F32)
        t1 = P.tile(128, F32)
        k1 = P.tile(1, F32)
        pidx = P.tile(1, F32)
        idb, idf = self.ident_bf, self.ident
        P.op(P.dve, lambda e: e.tensor_copy(idb.ap, idf.ap), reads=[idf], writes=[idb])
        P.op(P.pool, lambda e: e.iota(rel.ap, [[1, 128]], base=0, channel_multiplier=-1,
                                      allow_small_or_imprecise_dtypes=True), writes=[rel])
        P.op(P.pool, lambda e: e.iota(t1.ap, [[1, 128]], base=1, channel_multiplier=0,
                                      allow_small_or_imprecise_dtypes=True), writes=[t1])
        P.op(P.pool, lambda e: e.iota(k1.ap, [[0, 1]], base=127, channel_multiplier=-1,
                                      allow_small_or_imprecise_dtypes=True), writes=[k1])
        P.op(P.pool, lambda e: e.iota(pidx.ap, [[0, 1]], base=0, channel_multiplier=1,
                                      allow_small_or_imprecise_dtypes=True), writes=[pidx])
        pos0 = self.b_pos0
        P.op(P.pool, lambda e: e.iota(pos0.ap, [[1, 512]], base=0, channel_multiplier=0,
                                      allow_small_or_imprecise_dtypes=True), writes=[pos0])
        for h in range(4):
            lg = LOG_GAMMAS[h]
            self.act_fn(P.sub(self.b_DT, h * 128, (h + 1) * 128, F32), rel, AF.Exp, scale=lg)
            self.act_fn(P.sub(self.b_qdec, h * 128, (h + 1) * 128, F32), t1, AF.Exp, scale=lg)
            self.act_fn(P.sub(self.b_kdec, h, h + 1, F32), k1, AF.Exp, scale=lg)
        DT = self.b_DT
        P.op(P.pool, lambda e: e.affine_select(DT.ap, DT.ap, [[0, 4], [1, 128]], ALU.is_ge, 0.0, base=0,
                                               channel_multiplier=-1), reads=[DT], writes=[DT])
        self.act_fn(self.b_inv, pidx, AF.Exp, scale=-math.log(10000.0) / 128.0)
        for h in range(4):
            for half in range(2):
                S, Sb = self.S[h][half], self.Sbf[h][half]
                P.op(P.pool, lambda e, S=S: e.memset(S.ap, 0.0), writes=[S])
                P.op(P.pool, lambda e, Sb=Sb: e.memset(Sb.ap, 0.0), writes=[Sb])
        P.sp_off = mark

    def rotary_tables(self, ti, background=False):
        P = self.P
        PI = math.pi
        TWO_PI = 2.0 * math.pi
        C1 = 6.28125
        C2 = TWO_PI - C1
        cos_t, sin_t, cos16, sin16 = self.rot_tabs
        ang, tfix, rc = self.rot_tmp
        base = ti * self.T
        ops = []
        ops.append(lambda: self.ts(ang, self.b_pos0, float(base), self.b_inv.ap[:, 0:1], ALU.add, ALU.mult,
                                   extra_reads=[self.b_inv]))
        for b in (512, 256, 128, 64, 32, 16, 8, 4, 2, 1):
            ops.append(lambda b=b: self.ts(tfix, ang, float(b * TWO_PI), None, ALU.is_ge))
            ops.append(lambda b=b: self.stt(ang, tfix, -float(b * C1), ang, ALU.mult, ALU.add))
            ops.append(lambda b=b: self.stt(ang, tfix, -float(b * C2), ang, ALU.mult, ALU.add))
        ops.append(lambda: self.ts(ang, ang, -PI, None, ALU.add))
        ops.append(lambda: self.ts(tfix, ang, PI, TWO_PI, ALU.is_gt, ALU.mult))
        ops.append(lambda: self.tt(ang, ang, tfix, ALU.subtract))
        ops.append(lambda: self.ts(tfix, ang, -PI, TWO_PI, ALU.is_lt, ALU.mult))
        ops.append(lambda: self.tt(ang, ang, tfix, ALU.add))
        ops.append(lambda: self.ts(rc, ang, PI / 2.0, None, ALU.add))
        ops.append(lambda: self.ts(tfix, rc, PI, TWO_PI, ALU.is_gt, ALU.mult))
        ops.append(lambda: self.tt(rc, rc, tfix, ALU.subtract))
        ops.append(lambda: self.act_fn(sin_t, ang, AF.Sin, scale=-1.0))
        ops.append(lambda: self.act_fn(cos_t, rc, AF.Sin, scale=-1.0))
        ops.append(lambda: self.ts(sin16, sin_t, 1.0 / 16.0, None, ALU.mult))
        ops.append(lambda: self.ts(cos16, cos_t, 1.0 / 16.0, None, ALU.mult))
        if background:
            self.bg_ops.extend(ops)
        else:
            for f in ops:
                f()

    def bg_step(self, n=2):
        for _ in range(n):
            if self.bg_ops:
                self.bg_ops.pop(0)()

    def mixer_b(self, lnidx, ti):
        P = self.P
        NG = self.NG
        mark = P.sp_off
        w_in = self.b_w_in[0].rearrange("(k p) f -> p k f", p=128)
        w_out = self.b_w_out[0].rearrange("(f p) d -> p f d", p=128)
        PI = math.pi
        TWO_PI = 2.0 * math.pi
        C1 = 6.28125
        C2 = TWO_PI - C1
        pbanks = [0, 1, 2]

        def pbank():
            b = pbanks[self.mm_i % 3]
            self.mm_i += 1
            return b

        self.bg_step(1000)
        z_off = P.sp_off
        cos_t, sin_t, cos16, sin16 = self.rot_tabs
        ang, kf, tfix, rc = (P.tile(512, F32) for _ in range(4))
        ta, tb, tc, td = ang, kf, tfix, rc
        qT = [P.tile(512, BF16) for _ in range(2)]
        kT = [P.tile(512, BF16) for _ in range(2)]
        qd = [P.tile(512, BF16) for _ in range(2)]
        vh = [P.tile(512, BF16) for _ in range(4)]
        kd = [P.tile(256, BF16) for _ in range(4)]
        sc = [P.tile(128, BF16) for _ in range(2)]
        yh = P.tile(2048, F32)
        yb = [P.tile(512, BF16) for _ in range(4)]
        ysq = [P.tile(512, BF16) for _ in range(4)]
        gated = [[P.tile(512, BF16) for g in range(NG)] for f in range(16)]
        sgh = [P.tile(512, F32) for _ in range(4)]
        sci = 0
        for g in range(NG):
            def do_qk(h):
                qdec_b = self.b_qdec.ap[:, h * 128:(h + 1) * 128].unsqueeze(1).broadcast_to([128, 4, 128])
                wq = self.wload(w_in[:, :, h * 256:h * 256 + 256], [128, KD, 256])
                wk = self.wload(w_in[:, :, 1024 + h * 256:1024 + h * 256 + 256], [128, KD, 256])
                if h == 0 and NG == 1:
                    qkb = [P.bank(0), P.bank(1), P.bank(2), P.bank(4)]
                    self.kouter([(qkb[i],
                                  (lambda k, i=i: (wq if i < 2 else wk).ap[:, k, (i % 2) * 128:(i % 2 + 1) * 128]),
                                  (lambda k: self.xop[k][g].ap),
                                  (lambda k, i=i: [wq if i < 2 else wk, self.xop[k][g]])) for i in range(4)])
                else:
                    qkb = [P.bank(pbank()) for _ in range(3)] + [P.bank(self.next_y_bank())]
                    for i in range(4):
                        wb = wq if i < 2 else wk
                        for k in range(KD):
                            self.mm(qkb[i], wb.ap[:, k, (i % 2) * 128:(i % 2 + 1) * 128], self.xop[k][g].ap, k == 0,
                                    k == KD - 1, reads=[wb, self.xop[k][g]])
                for which in range(2):
                    b1, b2 = qkb[2 * which], qkb[2 * which + 1]
                    cs, sn = (cos_t, sin_t) if which == 0 else (cos16, sin16)
                    dst = qT if which == 0 else kT
                    self.tt(ta, b1, cs, ALU.mult)
                    self.tt(tc, b1, sn, ALU.mult)
                    self.tt(tb, b2, sn, ALU.mult)
                    self.tt(td, b2, cs, ALU.mult)
                    self.tt(dst[0], ta, tb, ALU.subtract)
                    self.tt(dst[1], tc, td, ALU.add)
                    if which == 0:
                        for half in range(2):
                            o3 = qd[half].ap.rearrange("p (c t) -> p c t", t=128)
                            i3 = qT[half].ap.rearrange("p (c t) -> p c t", t=128)
                            P.op(P.dve, lambda e, o3=o3, i3=i3, qdec_b=qdec_b: e.tensor_tensor(o3, i3, qdec_b, ALU.mult),
                                 reads=[qT[half], self.b_qdec], writes=[qd[half]])

            def do_vkt(h):
                wb = self.wload(w_in[:, :, 2048 + h * 512:2048 + (h + 1) * 512], [128, KD, 512])
                for c in range(4):
                    bk = P.bank(pbank())
                    for k in range(KD):
                        self.mm(bk, self.xop[k][g].ap[:, c * 128:(c + 1) * 128], wb.ap[:, k, :], k == 0, k == KD - 1,
                                reads=[wb, self.xop[k][g]])
                    self.act_fn(vh[c], bk, AF.Copy)
                for c in range(4):
                    b = pbank()
                    for half in range(2):
                        o = P.bank(b, half * 128, (half + 1) * 128)
                        self.mm(o, kT[half].ap[:, c * 128:(c + 1) * 128], self.ident_bf.ap, True, True,
                                reads=[kT[half], self.ident_bf], signal=(half == 1))
                    bk = P.bank(b, 0, 256)
                    self.act_fn(kd[c], bk, AF.Identity, scale=self.b_kdec.ap[:, h:h + 1], extra_reads=[self.b_kdec])

            def do_chunks(h):
                nonlocal sci
                wgb = self.wload(w_in[:, :, 4096 + h * 512:4096 + (h + 1) * 512], [128, KD, 512])
                yh3 = yh.ap.rearrange("p (v t) -> p v t", t=512)
                for c in range(4):
                    cs_ = slice(c * 128, (c + 1) * 128)
                    bs = P.bank(3, 0, 128)
                    for half in range(2):
                        self.mm(bs, kT[half].ap[:, cs_], qT[half].ap[:, cs_], half == 0, half == 1,
                                reads=[kT[half], qT[half]])
                    sct = sc[sci % 2]
                    sci += 1
                    dts = P.sub(self.b_DT, h * 128, (h + 1) * 128, F32)
                    self.tt(sct, bs, dts, ALU.mult)
                    bk = P.bank(pbank())
                    for k in range(KD):
                        self.mm(bk, wgb.ap[:, k, c * 128:(c + 1) * 128], self.xop[k][g].ap, k == 0, k == KD - 1,
                                reads=[wgb, self.xop[k][g]])
                    self.act_fn(sgh[c], bk, AF.Silu)
                    by_b = self.next_y_bank()
                    for vc in range(4):
                        o = P.bank(by_b, vc * 128, (vc + 1) * 128)
                        self.mm(o, vh[c].ap[:, vc * 128:(vc + 1) * 128], sct.ap, True, False, reads=[vh[c], sct], signal=False)
                        for half in range(2):
                            self.mm(o, self.Sbf[h][half].ap[:, vc * 128:(vc + 1) * 128], qd[half].ap[:, cs_], False, half == 1,
                                    reads=[self.Sbf[h][half], qd[half]], signal=(half == 1 and vc == 3))
                    by = P.bank(by_b)
                    o_ap = yh3[:, :, cs_]
                    i_ap = by.ap.rearrange("p (v t) -> p v t", t=128)
                    P.op(P.act, lambda e, o_ap=o_ap, i_ap=i_ap: e.activation(o_ap, i_ap, AF.Copy), reads=[by], writes=[yh])
                    for half in range(2):
                        bS = P.bank(6 + half)
                        self.mm(bS, kd[c].ap[:, half * 128:(half + 1) * 128], vh[c].ap, True, True, reads=[kd[c], vh[c]])
                        S, Sb = self.S[h][half], self.Sbf[h][half]
                        self.stt(S, S, GAMMAS[h] ** 128, bS, ALU.mult, ALU.add)
                        self.act_fn(Sb, S, AF.Copy)

            def do_gn(h):
                yv = [P.sub(yh, vc * 512, (vc + 1) * 512, F32) for vc in range(4)]
                for vc in range(4):
                    self.act_fn(yb[vc], yv[vc], AF.Copy)
                    self.act_fn(ysq[vc], yv[vc], AF.Square)
                s1, s2 = P.bank(6), P.bank(7)
                for vc in range(4):
                    self.mm(s1, self.ones_bf.ap, yb[vc].ap, vc == 0, vc == 3, reads=[self.ones_bf, yb[vc]])
                for vc in range(4):
                    self.mm(s2, self.ones_bf.ap, ysq[vc].ap, vc == 0, vc == 3, reads=[self.ones_bf, ysq[vc]])
                m, v, r = self.m_t, self.v_t, self.r_t
                self.ts(m, s1, 1.0 / 512, None, ALU.mult)
                self.tt(v, m, m, ALU.mult)
                self.stt(v, s2, 1.0 / 512, v, ALU.mult, ALU.subtract)
                self.act_fn(r, v, AF.Ln, bias=self.const_ap(GN_EPS), extra_reads=[self.cst])
                self.act_fn(r, r, AF.Exp, scale=-0.5)
                for vc in range(4):
                    self.tt(yv[vc], yv[vc], m, ALU.subtract)
                    self.tt(yv[vc], yv[vc], r, ALU.mult)
                    self.tt(gated[h * 4 + vc][g], yv[vc], sgh[vc], ALU.mult)

            do_qk(0)
            self.flush_pending()
            for h in range(4):
                do_vkt(h)
                do_chunks(h)
                if h < 3:
                    do_qk(h + 1)
                do_gn(h)
        wo_slots = {}

        def yfn(dc, g, bk):
            if g == 0 or dc not in wo_slots:
                wo_slots[dc] = self.wload(w_out[:, :, dc * 128:(dc + 1) * 128], [128, 16, 128])
            sd = wo_slots[dc]
            for f in range(16):
                self.mm(bk, sd.ap[:, f, :], gated[f][g].ap, f == 0, f == 15, reads=[sd, gated[f][g]])

        self.resid_ln(lnidx, ALPHA, LN_EPS, yfn, zoff=z_off)
        P.sp_off = mark


def _build(nsub=6, ntiles=None, T=512):
    k = Kern(nsub=nsub, ntiles=ntiles, T=T)
    return k


_CACHE = {}


def kernel(**inputs):
    x = np.ascontiguousarray(inputs["x"], dtype=np.float32)
    if "nc" not in _CACHE:
        k = Kern()
        _CACHE["nc"] = k.build()
    nc = _CACHE["nc"]
    names = ["ln_g", "ln_b", "ffn_w_gate", "ffn_w_up", "ffn_w_down", "a_w_in", "a_ln_g", "a_ln_b",
             "a_w_s", "a_b_s", "a_w_out", "b_w_in", "b_w_out"]
    shared = {n: np.ascontiguousarray(inputs[n], dtype=np.float32) for n in names}
    in_maps = []
    for b in range(NB):
        m = dict(shared)
        m["x"] = x[b]
        in_maps.append(m)
    res = run_bass_kernel_spmd(nc, in_maps, core_ids=list(range(NB)))
    return np.stack([r["out"] for r in res.results], axis=0)
```

```python
import math
from contextlib import ExitStack

import numpy as np
import concourse.bass as bass
import concourse.mybir as mybir
from concourse.bass_utils import run_bass_kernel_spmd

F32 = mybir.dt.float32
BF16 = mybir.dt.bfloat16
I32 = mybir.dt.int32
AF = mybir.ActivationFunctionType
ALU = mybir.AluOpType
AX = mybir.AxisListType

D = 1024
SEQ = 4096
NB = 8
DFF = 2816
KD = D // 128
KF = DFF // 128
DEPTH = 2
ALPHA = float((2 * DEPTH) ** 0.25)
LN_EPS = 1e-5
GN_EPS = 1e-6
CELL = 256
SLOT_BYTES = 8192
NSLOT = 4
GAMMAS = [1.0 - 2.0 ** (-5.0 - h) for h in range(4)]
LOG_GAMMAS = [math.log1p(-(2.0 ** (-5.0 - h))) for h in range(4)]


class Tl:
    def __init__(self, ap, space, c0, c1):
        self.ap = ap
        self.space = space
        self.c0 = c0
        self.c1 = c1

    def cells(self):
        return [(self.space, c) for c in range(self.c0, self.c1)]


class Eng:
    def __init__(self, prog, name, is_pe=False):
        self.prog = prog
        self.name = name
        self.is_pe = is_pe
        self.q = []
        self.sem = prog.new_sem(name)
        self.cnt = 0
        self.known = {}


class Prog:
    def __init__(self, nc, stack):
        self.nc = nc
        self.stack = stack
        self.sems = []
        self.cells = {}
        self.pe_sems = set()
        self.pe = Eng(self, "pe", True)
        self.pe_sems.add(self.pe.sem)
        self.act = Eng(self, "act")
        self.dve = Eng(self, "dve")
        self.pool = Eng(self, "pool")
        self.sp = Eng(self, "sp")
        self.dma_cnt = {}
        self.arena_bytes = 206 * 1024
        self.arena = stack.enter_context(nc.sbuf_tensor("arena", [128, self.arena_bytes // 4], F32))
        self.sp_off = 0
        self.psum = stack.enter_context(nc.psum_tensor("ps", [128, 8, 512], F32))

    def new_sem(self, name):
        h = self.stack.enter_context(self.nc.semaphore("s%d_%s" % (len(self.sems), name)))
        self.sems.append(h)
        return len(self.sems) - 1

    def alloc(self, nbytes):
        nbytes = (nbytes + CELL - 1) // CELL * CELL
        off = self.sp_off
        self.sp_off += nbytes
        self.hwm = max(getattr(self, "hwm", 0), self.sp_off)
        assert self.sp_off <= self.arena_bytes, "arena overflow %d" % self.sp_off
        return off

    def view(self, off, nelem, dt):
        esz = 4 if dt in (F32, I32) else 2
        nb = nelem * esz
        assert off % 4 == 0 and nb % 4 == 0
        ap = self.arena[:, off // 4:(off + nb) // 4]
        if dt != F32:
            ap = ap.bitcast(dt)
        return Tl(ap, "sb", off // CELL, (off + nb + CELL - 1) // CELL)

    def tile(self, nelem, dt):
        esz = 4 if dt in (F32, I32) else 2
        return self.view(self.alloc(nelem * esz), nelem, dt)

    def sub(self, tl, lo, hi, dt):
        esz = 4 if dt in (F32, I32) else 2
        base = tl.c0 * CELL
        return Tl(tl.ap[:, lo:hi], "sb", (base + lo * esz) // CELL, (base + hi * esz + CELL - 1) // CELL)

    def bank(self, b, lo=0, hi=512):
        return Tl(self.psum[:, b, lo:hi], "ps", b, b + 1)

    def op(self, eng, fn, reads=(), writes=(), signal=True, dma_sem=None):
        deps = {}
        cells = self.cells
        ps_reads = [t for t in reads if t.space == "ps"]
        if ps_reads:
            reads = [t for t in reads if t.space != "ps"]
            writes = list(writes) + ps_reads
        for t in reads:
            for c in t.cells():
                st = cells.get(c)
                if st is not None and st[0] is not None:
                    s, v = st[0]
                    if deps.get(s, 0) < v:
                        deps[s] = v
        for t in writes:
            for c in t.cells():
                st = cells.get(c)
                if st is not None:
                    if st[0] is not None:
                        s, v = st[0]
                        if deps.get(s, 0) < v:
                            deps[s] = v
                    for s, v in st[1].items():
                        if deps.get(s, 0) < v:
                            deps[s] = v
        waits = []
        for s, v in deps.items():
            if eng.is_pe and s in self.pe_sems:
                continue
            if eng.known.get(s, 0) >= v:
                continue
            eng.known[s] = v
            waits.append((s, v))
        if dma_sem is not None:
            self.dma_cnt[dma_sem] = self.dma_cnt.get(dma_sem, 0) + 16
            evt = (dma_sem, self.dma_cnt[dma_sem])
            inc = (dma_sem, 16)
        else:
            evt = (eng.sem, eng.cnt + 1)
            if signal:
                eng.cnt += 1
                inc = (eng.sem, 1)
            else:
                inc = None
        for t in reads:
            for c in t.cells():
                st = cells.get(c)
                if st is None:
                    st = cells[c] = [None, {}]
                if st[1].get(evt[0], 0) < evt[1]:
                    st[1][evt[0]] = evt[1]
        for t in writes:
            for c in t.cells():
                cells[c] = [evt, {}]
        eng.q.append((waits, fn, inc))
        return evt

    def emit(self):
        nc = self.nc
        sems = self.sems

        def run(q):
            def body(e):
                for waits, fn, inc in q:
                    for s, v in waits:
                        e.wait_ge(sems[s], v)
                    ins = fn(e)
                    if inc is not None:
                        ins.then_inc(sems[inc[0]], inc[1])
            return body

        with nc.Block() as block:
            block.tensor(run(self.pe.q))
            block.scalar(run(self.act.q))
            block.vector(run(self.dve.q))
            block.gpsimd(run(self.pool.q))
            block.sync(run(self.sp.q))


class Kern:
    def __init__(self, nsub=6, ntiles=None, T=512):
        self.T = T
        self.NG = T // 512
        self.ntiles = ntiles if ntiles is not None else SEQ // T
        self.nsub = nsub
        self.nc = nc = bass.Bass("TRN2", target_bir_lowering=False)
        dt = nc.dram_tensor
        self.x = dt("x", [SEQ, D], F32, kind="ExternalInput").ap()
        self.ln_g = dt("ln_g", [2, 3, D], F32, kind="ExternalInput").ap()
        self.ln_b = dt("ln_b", [2, 3, D], F32, kind="ExternalInput").ap()
        self.wg = dt("ffn_w_gate", [2, 2, D, DFF], F32, kind="ExternalInput").ap()
        self.wu = dt("ffn_w_up", [2, 2, D, DFF], F32, kind="ExternalInput").ap()
        self.wd = dt("ffn_w_down", [2, 2, DFF, D], F32, kind="ExternalInput").ap()
        self.a_w_in = dt("a_w_in", [1, D, 4096], F32, kind="ExternalInput").ap()
        self.a_ln_g = dt("a_ln_g", [1, 2048], F32, kind="ExternalInput").ap()
        self.a_ln_b = dt("a_ln_b", [1, 2048], F32, kind="ExternalInput").ap()
        self.a_w_s = dt("a_w_s", [1, 8, 128, 128], F32, kind="ExternalInput").ap()
        self.a_b_s = dt("a_b_s", [1, 8, 128], F32, kind="ExternalInput").ap()
        self.a_w_out = dt("a_w_out", [1, 2048, D], F32, kind="ExternalInput").ap()
        self.b_w_in = dt("b_w_in", [1, D, 6144], F32, kind="ExternalInput").ap()
        self.b_w_out = dt("b_w_out", [1, 2048, D], F32, kind="ExternalInput").ap()
        self.out = dt("out", [SEQ, D], F32, kind="ExternalOutput").ap()

    def mm(self, out, lhsT, rhs, start, stop, reads, signal=None):
        P = self.P
        if signal is None:
            signal = stop
        o, l, r = out.ap, lhsT, rhs
        P.op(P.pe, lambda e: e.matmul(o, l, r, start=start, stop=stop), reads=reads, writes=[out], signal=signal)

    def act_fn(self, out, in_, func, reads=None, bias=None, scale=None, extra_reads=(), accum=None):
        P = self.P
        kw = {}
        if bias is not None:
            kw["bias"] = bias
        if scale is not None:
            kw["scale"] = scale
        wr = [out]
        if accum is not None:
            kw["accum_out"] = accum.ap
            wr.append(accum)
        o, i = out.ap, in_.ap
        P.op(P.act, lambda e: e.activation(o, i, func, **kw), reads=[in_] + list(extra_reads), writes=wr)

    def tt(self, out, in0, in1, op, eng=None):
        P = self.P
        eng = eng or P.dve
        o, a, b = out.ap, in0.ap, in1.ap
        P.op(eng, lambda e: e.tensor_tensor(o, a, b, op), reads=[in0, in1], writes=[out])

    def ts(self, out, in0, s1, s2, op0, op1=None, extra_reads=(), eng=None, accum=None):
        P = self.P
        eng = eng or P.dve
        o, a = out.ap, in0.ap
        wr = [out]
        kw = {}
        if accum is not None:
            kw["accum_out"] = accum.ap
            wr.append(accum)
        if op1 is None:
            P.op(eng, lambda e: e.tensor_scalar(o, a, s1, None, op0, **kw), reads=[in0] + list(extra_reads), writes=wr)
        else:
            P.op(eng, lambda e: e.tensor_scalar(o, a, s1, s2, op0, op1, **kw), reads=[in0] + list(extra_reads), writes=wr)

    def stt(self, out, in0, scalar, in1, op0, op1, extra_reads=(), accum=None):
        P = self.P
        o, a, b = out.ap, in0.ap, in1.ap
        wr = [out]
        kw = {}
        if accum is not None:
            kw["accum_out"] = accum.ap
            wr.append(accum)
        P.op(P.dve, lambda e: e.scalar_tensor_tensor(o, a, scalar, b, op0, op1, **kw),
             reads=[in0, in1] + list(extra_reads), writes=wr)

    def wload(self, src_ap, shape):
        P = self.P
        s = self.slot_i % NSLOT
        self.slot_i += 1
        n = 1
        for d in shape[1:]:
            n *= d
        assert n * 2 <= SLOT_BYTES
        full = self.slots[s]
        v = full.ap[:, 0:n]
        if len(shape) == 3:
            v = v.rearrange("p (a b) -> p a b", b=shape[2])
        tl = Tl(v, "sb", full.c0, full.c1)
        P.op(P.pool, lambda e: e.dma_start(out=v, in_=src_ap), reads=[], writes=[tl], dma_sem=self.slot_sems[s])
        return tl

    def build(self):
        nc = self.nc
        with ExitStack() as stack:
            self.P = P = Prog(nc, stack)
            T, NG = self.T, self.NG
            self.slots = [P.tile(SLOT_BYTES // 2, BF16) for _ in range(NSLOT)]
            self.slot_sems = [P.new_sem("slot%d" % i) for i in range(NSLOT)]
            self.slot_i = 0
            self.in_sems = [P.new_sem("in%d" % i) for i in range(4)]
            self.out_sems = [P.new_sem("out%d" % i) for i in range(4)]
            self.c_sem = P.new_sem("const")
            self.xres = [[P.tile(512, F32) for g in range(NG)] for dc in range(KD)]
            self.xop = [[P.tile(512, BF16) for g in range(NG)] for dc in range(KD)]
            self.ident = P.tile(128, F32)
            self.ones_bf = P.tile(128, BF16)
            self.cst = P.tile(64, F32)
            self.dummy = P.tile(64, F32)
            self.next_func = None
            self.pending = []
            self.bg_ops = []
            self.last_sub = False
            self.cst_cols = {}
            self.lngb = P.tile(12 * KD, F32)
            self.lng = P.sub(self.lngb, 0, 6 * KD, F32)
            self.lnb = P.sub(self.lngb, 6 * KD, 12 * KD, F32)
            self.m_t = P.tile(512, F32)
            self.v_t = P.tile(512, F32)
            self.r_t = P.tile(512, F32)
            self.sg_t = [P.tile(512, F32) for _ in range(2)]
            self.sg_i = 0
            self.scratch_base = P.sp_off
            self.mm_banks = [0, 1, 2, 3]
            self.mm_i = 0
            self.y_banks = [4, 5]
            self.y_i = 0

            if self.nsub >= 2:
                self.setup_mixer_a()
            if self.nsub >= 5:
                self.setup_mixer_b()
            self.scratch_base = P.sp_off
            self.load_dma(0)
            self.setup_consts()
            for ti in range(self.ntiles):
                self.load_tile(ti)
                si = 0
                for li in range(DEPTH):
                    for part in range(3):
                        if si >= self.nsub:
                            break
                        last = (si == self.nsub - 1)
                        if last and ti + 1 < self.ntiles and part != 1:
                            self.load_dma(ti + 1)
                        self.next_func = AF.Gelu if (li == 0 and part == 0) else AF.Silu
                        self.last_sub = last
                        if part == 0:
                            if li == 1 and ti > 0 and self.nsub >= 5:
                                self.rotary_tables(ti, background=True)
                            self.ffn(li, 0, li * 3 + 0)
                        elif part == 1:
                            if li == 0:
                                if ti == 0:
                                    self.emit_mixer_a()
                                self.mixer_a(li * 3 + 1, ti)
                            else:
                                if ti == 0:
                                    self.emit_mixer_b()
                                    self.rotary_tables(0)
                                self.mixer_b(li * 3 + 1, ti)
                        else:
                            self.ffn(li, 1, li * 3 + 2)
                        si += 1
                if ti + 1 < self.ntiles and (self.nsub == 0 or (self.nsub - 1) % 3 == 1):
                    self.load_dma(ti + 1)
                self.store_tile(ti)
            P.sp.q.append(([(s_, P.dma_cnt.get(s_, 0)) for s_ in self.out_sems], lambda e: e.nop(), None))
            P.emit()
        return nc

    def kouter(self, accs):
        for k in range(KD):
            for (bk, lf, rf, rdf) in accs:
                self.mm(bk, lf(k), rf(k), k == 0, k == KD - 1, reads=rdf(k))

    def next_mm_bank(self):
        b = self.mm_banks[self.mm_i % len(self.mm_banks)]
        self.mm_i += 1
        return b

    def next_y_bank(self):
        b = self.y_banks[self.y_i % len(self.y_banks)]
        self.y_i += 1
        return b

    def setup_consts(self):
        P = self.P
        nc = self.nc
        idt = self.ident
        P.op(P.pool, lambda e: e.memset(idt.ap, 1.0), writes=[idt])
        P.op(P.pool, lambda e: e.affine_select(idt.ap, idt.ap, [[1, 128]], ALU.is_equal, 0.0, base=0,
                                               channel_multiplier=-1), reads=[idt], writes=[idt])
        ob = self.ones_bf
        P.op(P.pool, lambda e: e.memset(ob.ap, 1.0), writes=[ob])
        for l in range(2):
            for j in range(3):
                idx = (l * 3 + j) * KD
                for (dst, src) in ((self.lng, self.ln_g), (self.lnb, self.ln_b)):
                    d_ap = dst.ap[:, idx:idx + KD]
                    s_ap = src[l, j].rearrange("(c p) -> p c", p=128)
                    P.op(P.sp, lambda e, d_ap=d_ap, s_ap=s_ap: e.dma_start(out=d_ap, in_=s_ap,
                                                                          allow_slow_non_contiguous=True),
                         writes=[dst], dma_sem=self.c_sem)

    def load_dma(self, ti):
        P = self.P
        self.in_st = []
        for g in range(self.NG):
            st = [P.view(self.scratch_base + 40960 + (g * 4 + c) * 4096, D, F32) for c in range(4)]
            self.in_st.append(st)
            for c in range(4):
                r0 = ti * self.T + g * 512 + c * 128
                src = self.x[r0:r0 + 128, :]
                d_ap = st[c].ap
                P.op(P.sp, lambda e, d_ap=d_ap, src=src: e.dma_start(out=d_ap, in_=src), writes=[st[c]],
                     dma_sem=self.in_sems[c])

    def load_tile(self, ti):
        P = self.P
        for g in range(self.NG):
            st = self.in_st[g]
            for dc in range(KD):
                b = self.next_mm_bank()
                for c in range(4):
                    o = P.bank(b, c * 128, (c + 1) * 128)
                    i_ap = st[c].ap[:, dc * 128:(dc + 1) * 128]
                    idt = self.ident
                    P.op(P.pe, lambda e, o=o, i_ap=i_ap, idt=idt: e.transpose(o.ap, i_ap, idt.ap),
                         reads=[st[c], idt], writes=[o], signal=(c == 3))
                bk = P.bank(b)
                xr, xo = self.xres[dc][g], self.xop[dc][g]
                P.op(P.act, lambda e, xr=xr, bk=bk: e.activation(xr.ap, bk.ap, AF.Copy), reads=[bk], writes=[xr])
                P.op(P.dve, lambda e, xo=xo, xr=xr: e.tensor_copy(xo.ap, xr.ap), reads=[xr], writes=[xo])

    def store_tile(self, ti):
        P = self.P
        self.flush_pending()
        mark = P.sp_off
        for g in range(self.NG):
            st = [P.tile(D, F32) for _ in range(4)]
            for c in range(4):
                for half in range(2):
                    b = self.next_mm_bank()
                    for q in range(4):
                        dc = half * 4 + q
                        o = P.bank(b, q * 128, (q + 1) * 128)
                        xr = self.xres[dc][g]
                        i_ap = xr.ap[:, c * 128:(c + 1) * 128]
                        idt = self.ident
                        P.op(P.pe, lambda e, o=o, i_ap=i_ap, idt=idt: e.transpose(o.ap, i_ap, idt.ap),
                             reads=[xr, idt], writes=[o], signal=(q == 3))
                    bk = P.bank(b)
                    dst = P.sub(st[c], half * 512, (half + 1) * 512, F32)
                    if half == 0:
                        P.op(P.act, lambda e, dst=dst, bk=bk: e.activation(dst.ap, bk.ap, AF.Copy), reads=[bk], writes=[dst])
                    else:
                        P.op(P.dve, lambda e, dst=dst, bk=bk: e.tensor_copy(dst.ap, bk.ap), reads=[bk], writes=[dst])
                r0 = ti * self.T + g * 512 + c * 128
                dst_ap = self.out[r0:r0 + 128, :]
                s_ap = st[c].ap
                P.op(P.sp, lambda e, dst_ap=dst_ap, s_ap=s_ap: e.dma_start(out=dst_ap, in_=s_ap), reads=[st[c]],
                     dma_sem=self.out_sems[c])
        P.sp_off = mark

    def ln_stats_mm(self, g, dc, s1, s2):
        self.mm(s1, self.ones_bf.ap, self.zb[dc].ap, dc == 0, dc == KD - 1, reads=[self.ones_bf, self.zb[dc]])
        self.mm(s2, self.ones_bf.ap, self.zsq[dc].ap, dc == 0, dc == KD - 1, reads=[self.ones_bf, self.zsq[dc]])

    def ln_finalize(self, g, lnidx, eps, s1, s2):
        P = self.P
        m, v, r = self.m_t, self.v_t, self.r_t
        self.ts(m, s1, 1.0 / D, None, ALU.mult)
        self.tt(v, m, m, ALU.mult)
        self.stt(v, s2, 1.0 / D, v, ALU.mult, ALU.subtract)
        self.act_fn(r, v, AF.Ln, bias=self.const_ap(eps), extra_reads=[self.cst])
        self.act_fn(r, r, AF.Exp, scale=-0.5)
        gb = []
        for dc in range(KD):
            z = self.xres[dc][g]
            self.tt(z, z, m, ALU.subtract)
            self.tt(z, z, r, ALU.mult)
            gi = lnidx * KD + dc
            g_ap = self.lng.ap[:, gi:gi + 1]
            b_ap = self.lnb.ap[:, gi:gi + 1]
            gb.append((g_ap, b_ap))
            if self.last_sub:
                self.act_fn(z, z, AF.Identity, bias=b_ap, scale=g_ap, extra_reads=[self.lng, self.lnb])
                continue
            self.act_fn(self.xop[dc][g], z, AF.Identity, bias=b_ap, scale=g_ap, extra_reads=[self.lng, self.lnb])
        if self.last_sub:
            return
        if self.next_func is not None:
            self.act_fn(P.sub(self.dummy, 0, 1, F32), self.const_tl(1.0), self.next_func)

        def finish(dc_list=tuple(range(KD)), g=g, gb=gb):
            for dc in dc_list:
                z = self.xres[dc][g]
                g_ap, b_ap = gb[dc]
                self.act_fn(z, z, AF.Identity, bias=b_ap, scale=g_ap, extra_reads=[self.lng, self.lnb])

        self.pending.append(finish)

    def flush_pending(self):
        pend, self.pending = self.pending, []
        for f in pend:
            f()

    def const_tl(self, val):
        self.const_ap(val)
        i = self.cst_cols[val]
        return self.P.sub(self.cst, i, i + 1, F32)

    def const_ap(self, val):
        P = self.P
        if val not in self.cst_cols:
            i = len(self.cst_cols)
            assert i < 16
            self.cst_cols[val] = i
            col = self.cst.ap[:, i:i + 1]
            P.op(P.pool, lambda e: e.memset(col, float(val)), writes=[self.cst])
        i = self.cst_cols[val]
        return self.cst.ap[:, i:i + 1]

    def resid_ln(self, lnidx, c_res, eps, ybank_fn, zoff=None):
        P = self.P
        self.flush_pending()
        if zoff is None:
            self.zb = [P.tile(512, BF16) for _ in range(KD)]
            self.zsq = [P.tile(512, BF16) for _ in range(KD)]
        else:
            self.zb = [P.view(zoff + i * 1024, 512, BF16) for i in range(KD)]
            self.zsq = [P.view(zoff + 8192 + i * 1024, 512, BF16) for i in range(KD)]
        for g in range(self.NG):
            s1, s2 = P.bank(6), P.bank(7)
            prev = None
            self.act_fn(P.sub(self.dummy, 0, 1, F32), self.const_tl(1.0), AF.Ln, bias=self.const_ap(1.0), extra_reads=[self.cst])
            for dc in range(KD):
                bk = P.bank(self.next_y_bank())
                ybank_fn(dc, g, bk)
                if prev is not None:
                    self.ln_stats_mm(g, prev, s1, s2)
                z = self.xres[dc][g]
                self.stt(z, z, c_res, bk, ALU.mult, ALU.add)
                self.act_fn(self.zb[dc], z, AF.Copy)
                self.act_fn(self.zsq[dc], z, AF.Square)
                prev = dc
            self.ln_stats_mm(g, prev, s1, s2)
            self.ln_finalize(g, lnidx, eps, s1, s2)

    def ffn(self, li, j, lnidx):
        P = self.P
        NG = self.NG
        mark = P.sp_off
        h = [[P.tile(512, BF16) for g in range(NG)] for f in range(KF)]
        wg = self.wg[li, j].rearrange("(k p) f -> p k f", p=128)
        wu = self.wu[li, j].rearrange("(k p) f -> p k f", p=128)
        wd = self.wd[li, j].rearrange("(f p) d -> p f d", p=128)
        first = True
        for c0 in range(0, DFF, 512):
            n = min(512, DFF - c0)
            sg = self.wload(wg[:, :, c0:c0 + n], [128, KD, n])
            su = self.wload(wu[:, :, c0:c0 + n], [128, KD, n])
            fls = list(range(n // 128))
            if first and NG == 1:
                first = False
                g = 0
                accs = []
                for fl in fls[:3]:
                    for (w, b) in ((sg, 2 * fl), (su, 2 * fl + 1)):
                        accs.append((P.bank(b),
                                     (lambda k, w=w, fl=fl: w.ap[:, k, fl * 128:(fl + 1) * 128]),
                                     (lambda k: self.xop[k][g].ap),
                                     (lambda k, w=w: [w, self.xop[k][g]])))
                self.kouter(accs)
                for fl in fls[:3]:
                    tmp = self.sg_t[self.sg_i % 2]
                    self.sg_i += 1
                    self.act_fn(tmp, P.bank(2 * fl), AF.Silu)
                    self.tt(h[c0 // 128 + fl][g], tmp, P.bank(2 * fl + 1), ALU.mult)
                self.flush_pending()
                fls = fls[3:]
            for fl in fls:
                fc = c0 // 128 + fl
                for g in range(NG):
                    bg = P.bank(self.next_mm_bank())
                    bu = P.bank(self.next_mm_bank())
                    for k in range(KD):
                        self.mm(bg, sg.ap[:, k, fl * 128:(fl + 1) * 128], self.xop[k][g].ap, k == 0, k == KD - 1,
                                reads=[sg, self.xop[k][g]])
                    for k in range(KD):
                        self.mm(bu, su.ap[:, k, fl * 128:(fl + 1) * 128], self.xop[k][g].ap, k == 0, k == KD - 1,
                                reads=[su, self.xop[k][g]])
                    tmp = self.sg_t[self.sg_i % 2]
                    self.sg_i += 1
                    self.act_fn(tmp, bg, AF.Silu)
                    self.tt(h[fc][g], tmp, bu, ALU.mult)
                    self.bg_step(2)

        wd_slots = {}

        def yfn(dc, g, bk):
            if g == 0 or dc not in wd_slots:
                wd_slots[dc] = self.wload(wd[:, :, dc * 128:(dc + 1) * 128], [128, KF, 128])
            sd = wd_slots[dc]
            for f in range(KF):
                self.mm(bk, sd.ap[:, f, :], h[f][g].ap, f == 0, f == KF - 1, reads=[sd, h[f][g]])

        self.resid_ln(lnidx, 2.0 * ALPHA, 4.0 * LN_EPS, yfn)
        P.sp_off = mark

    def setup_mixer_a(self):
        P = self.P
        self.a_wsT = P.tile(8 * 128, BF16)
        self.a_blhs = P.tile(2048, BF16)
        self.a_brhs = P.tile(1024, BF16)
        self.a_G = P.tile(2048, F32)
        self.a_sem = P.new_sem("a_const")

    def emit_mixer_a(self):
        P = self.P
        mark = P.sp_off
        wst = P.tile(1024, F32)
        wT = P.tile(1024, F32)
        bs_f = P.tile(1024, F32)
        bs_h = P.tile(1024, BF16)
        bs_l = P.tile(1024, BF16)
        ones_f = P.tile(1, F32)
        csem = self.a_sem
        src = self.a_w_s[0].rearrange("g t s -> t g s")
        d3 = wst.ap.rearrange("p (g s) -> p g s", s=128)
        P.op(P.sp, lambda e: e.dma_start(out=d3, in_=src), writes=[wst], dma_sem=P.new_sem("ac"))
        P.op(P.pool, lambda e: e.memset(ones_f.ap, 1.0), writes=[ones_f])
        for half in range(2):
            b = self.next_mm_bank()
            for q in range(4):
                g = half * 4 + q
                o = P.bank(b, q * 128, (q + 1) * 128)
                i_ap = wst.ap[:, g * 128:(g + 1) * 128]
                idt = self.ident
                P.op(P.pe, lambda e, o=o, i_ap=i_ap, idt=idt: e.transpose(o.ap, i_ap, idt.ap), reads=[wst, idt],
                     writes=[o], signal=(q == 3))
            bk = P.bank(b)
            dst = P.sub(wT, half * 512, (half + 1) * 512, F32)
            P.op(P.act, lambda e, dst=dst, bk=bk: e.activation(dst.ap, bk.ap, AF.Copy), reads=[bk], writes=[dst])
        P.op(P.pool, lambda e: e.affine_select(wT.ap, wT.ap, [[0, 8], [1, 128]], ALU.is_ge, 0.0, base=0,
                                               channel_multiplier=-1), reads=[wT], writes=[wT])
        wsT = self.a_wsT
        P.op(P.dve, lambda e: e.tensor_copy(wsT.ap, wT.ap), reads=[wT], writes=[wsT])
        brhs = self.a_brhs
        for half in range(2):
            bk = P.bank(self.next_mm_bank())
            o_ap = bk.ap[0:1, :]
            r_ap = wT.ap[:, half * 512:(half + 1) * 512]
            l_ap = ones_f.ap
            P.op(P.pe, lambda e, o_ap=o_ap, l_ap=l_ap, r_ap=r_ap: e.matmul(o_ap, l_ap, r_ap, start=True, stop=True),
                 reads=[ones_f, wT], writes=[bk])
            d_ap = brhs.ap[0:1, half * 512:(half + 1) * 512]
            P.op(P.act, lambda e, d_ap=d_ap, o_ap=o_ap: e.activation(d_ap, o_ap, AF.Copy), reads=[bk], writes=[brhs])
        bsrc = self.a_b_s[0:1].rearrange("o g t -> o (g t)")
        P.op(P.sp, lambda e: e.dma_start(out=bs_f.ap[0:1, :], in_=bsrc), writes=[bs_f], dma_sem=P.new_sem("ac"))
        P.op(P.dve, lambda e: e.tensor_copy(bs_h.ap[0:1, :], bs_f.ap[0:1, :]), reads=[bs_f], writes=[bs_h])
        P.op(P.dve, lambda e: e.tensor_tensor(bs_l.ap[0:1, :], bs_f.ap[0:1, :], bs_h.ap[0:1, :], ALU.subtract),
             reads=[bs_f, bs_h], writes=[bs_l])
        P.op(P.sp, lambda e: e.dma_start(out=brhs.ap[1:2, :], in_=bs_h.ap[0:1, :]), reads=[bs_h], writes=[brhs],
             dma_sem=P.new_sem("ac"))
        P.op(P.sp, lambda e: e.dma_start(out=brhs.ap[2:3, :], in_=bs_l.ap[0:1, :]), reads=[bs_l], writes=[brhs],
             dma_sem=P.new_sem("ac"))
        blhs = self.a_blhs
        P.op(P.pool, lambda e: e.memset(blhs.ap[0:3, :], 1.0), writes=[blhs])
        P.op(P.pool, lambda e: e.dma_start(out=blhs.ap[0:1, :], in_=self.a_ln_b[0:1, :]), writes=[blhs],
             dma_sem=P.new_sem("ac"))
        G = self.a_G
        gsrc = self.a_ln_g[0:1, :].partition_broadcast(128)
        g3 = G.ap.rearrange("p (o f) -> p o f", o=1)
        P.op(P.sp, lambda e: e.dma_start(out=g3, in_=gsrc), writes=[G], dma_sem=P.new_sem("ac"))
        P.sp_off = mark

    def mixer_a(self, lnidx, ti):
        P = self.P
        NG = self.NG
        mark = P.sp_off
        w_in = self.a_w_in[0].rearrange("(k p) f -> p k f", p=128)
        w_out = self.a_w_out[0].rearrange("(f p) d -> p f d", p=128)
        vt_off = P.sp_off
        vt = [[P.tile(2048, F32) for c in range(4)] for g in range(NG)]
        vln = [[P.tile(2048, BF16) for c in range(4)] for g in range(NG)]
        ut = [[(P.tile(512, F32) if f < 4 else P.view(vt_off + g * 32768 + (f - 4) * 2048, 512, F32))
               for f in range(16)] for g in range(NG)]
        gated = [[P.tile(512, BF16) for g in range(NG)] for f in range(16)]
        s1 = [P.tile(16, F32) for g in range(NG)]
        s2 = [P.tile(16, F32) for g in range(NG)]
        st4 = [P.tile(16, F32) for g in range(NG)]
        junk = [P.tile(512, F32) for _ in range(2)]
        for vg in range(4):
            wb = self.wload(w_in[:, :, 2048 + vg * 512:2048 + (vg + 1) * 512], [128, KD, 512])
            for g in range(NG):
                kout = (vg == 0 and NG == 1)
                if kout:
                    self.kouter([(P.bank(c),
                                  (lambda k, c=c: self.xop[k][g].ap[:, c * 128:(c + 1) * 128]),
                                  (lambda k, wb=wb: wb.ap[:, k, :]),
                                  (lambda k, wb=wb: [wb, self.xop[k][g]])) for c in range(4)])
                for c in range(4):
                    if kout:
                        bk = P.bank(c)
                    else:
                        bk = P.bank(self.next_mm_bank())
                        for k in range(KD):
                            self.mm(bk, self.xop[k][g].ap[:, c * 128:(c + 1) * 128], wb.ap[:, k, :], k == 0, k == KD - 1,
                                    reads=[wb, self.xop[k][g]])
                    vsl = P.sub(vt[g][c], vg * 512, (vg + 1) * 512, F32)
                    a1 = P.sub(s1[g], c * 4 + vg, c * 4 + vg + 1, F32)
                    self.act_fn(vsl, bk, AF.Gelu, accum=a1)
                    a2 = P.sub(s2[g], c * 4 + vg, c * 4 + vg + 1, F32)
                    jk = junk[(c + vg) % 2]
                    self.stt(jk, vsl, 1.0, vsl, ALU.mult, ALU.mult, accum=a2)
        self.flush_pending()
        for ug in range(4):
            wb = self.wload(w_in[:, :, ug * 512:(ug + 1) * 512], [128, KD, 512])
            for fl in range(4):
                fc = ug * 4 + fl
                for g in range(NG):
                    bk = P.bank(self.next_mm_bank())
                    for k in range(KD):
                        self.mm(bk, wb.ap[:, k, fl * 128:(fl + 1) * 128], self.xop[k][g].ap, k == 0, k == KD - 1,
                                reads=[wb, self.xop[k][g]])
                    self.act_fn(ut[g][fc], bk, AF.Gelu)
            if ug == 0:
                for g in range(NG):
                    st = st4[g]
                    mean = P.sub(st, 0, 4, F32)
                    e2 = P.sub(st, 4, 8, F32)
                    var = P.sub(st, 8, 12, F32)
                    tmp = P.sub(st, 12, 16, F32)
                    s1v = s1[g].ap.rearrange("p (c v) -> p c v", v=4)
                    s2v = s2[g].ap.rearrange("p (c v) -> p c v", v=4)
                    P.op(P.dve, lambda e, mean=mean, s1v=s1v: e.tensor_reduce(mean.ap, s1v, AX.X, ALU.add),
                         reads=[s1[g]], writes=[mean])
                    P.op(P.dve, lambda e, e2=e2, s2v=s2v: e.tensor_reduce(e2.ap, s2v, AX.X, ALU.add),
                         reads=[s2[g]], writes=[e2])
                    self.ts(mean, mean, 1.0 / 2048, None, ALU.mult)
                    self.tt(tmp, mean, mean, ALU.mult)
                    self.stt(var, e2, 1.0 / 2048, tmp, ALU.mult, ALU.subtract)
                    self.act_fn(var, var, AF.Sqrt, bias=self.const_ap(LN_EPS), extra_reads=[self.cst])
                    P.op(P.dve, lambda e, var=var: e.reciprocal(var.ap, var.ap), reads=[var], writes=[var])
                    for c in range(4):
                        m_ap = mean.ap[:, c:c + 1]
                        r_ap = var.ap[:, c:c + 1]
                        self.ts(vt[g][c], vt[g][c], m_ap, r_ap, ALU.subtract, ALU.mult, extra_reads=[st])
                        self.tt(vln[g][c], vt[g][c], self.a_G, ALU.mult)
        for fc in range(16):
            G8 = fc // 2
            for g in range(NG):
                bk = P.bank(self.next_mm_bank())
                for c in range(4):
                    o = P.bank(bk.c0, c * 128, (c + 1) * 128)
                    self.mm(o, vln[g][c].ap[:, fc * 128:(fc + 1) * 128], self.a_wsT.ap[:, G8 * 128:(G8 + 1) * 128],
                            True, False, reads=[vln[g][c], self.a_wsT], signal=False)
                    self.mm(o, self.a_blhs.ap[0:3, fc * 128:(fc + 1) * 128], self.a_brhs.ap[0:3, G8 * 128:(G8 + 1) * 128],
                            False, True, reads=[self.a_blhs, self.a_brhs], signal=(c == 3))
                self.tt(gated[fc][g], ut[g][fc], bk, ALU.mult)
        wo_slots = {}

        def yfn(dc, g, bk):
            if g == 0 or dc not in wo_slots:
                wo_slots[dc] = self.wload(w_out[:, :, dc * 128:(dc + 1) * 128], [128, 16, 128])
            sd = wo_slots[dc]
            for f in range(16):
                self.mm(bk, sd.ap[:, f, :], gated[f][g].ap, f == 0, f == 15, reads=[sd, gated[f][g]])

        self.resid_ln(lnidx, ALPHA, LN_EPS, yfn, zoff=vt_off + 24576 if NG == 1 else None)
        P.sp_off = mark

    def setup_mixer_b(self):
        P = self.P
        self.b_DT = P.tile(512, F32)
        self.b_qdec = P.tile(512, F32)
        self.b_kdec = P.tile(4, F32)
        self.b_pos0 = P.tile(512, F32)
        self.b_inv = P.tile(1, F32)
        self.ident_bf = P.tile(128, BF16)
        self.rot_tabs = [P.tile(512, F32) for _ in range(4)]
        self.rot_tmp = [P.tile(512, F32) for _ in range(3)]
        self.S = [[P.tile(512, F32) for half in range(2)] for h in range(4)]
        self.Sbf = [[P.tile(512, BF16) for half in range(2)] for h in range(4)]

    def emit_mixer_b(self):
        P = self.P
        mark = P.sp_off
        rel = P.tile(128, F32)
        t1 = P.tile(128, F32)
        k1 = P.tile(1, F32)
        pidx = P.tile(1, F32)
        idb, idf = self.ident_bf, self.ident
        P.op(P.dve, lambda e: e.tensor_copy(idb.ap, idf.ap), reads=[idf], writes=[idb])
        P.op(P.pool, lambda e: e.iota(rel.ap, [[1, 128]], base=0, channel_multiplier=-1,
                                      allow_small_or_imprecise_dtypes=True), writes=[rel])
        P.op(P.pool, lambda e: e.iota(t1.ap, [[1, 128]], base=1, channel_multiplier=0,
                                      allow_small_or_imprecise_dtypes=True), writes=[t1])
        P.op(P.pool, lambda e: e.iota(k1.ap, [[0, 1]], base=127, channel_multiplier=-1,
                                      allow_small_or_imprecise_dtypes=True), writes=[k1])
        P.op(P.pool, lambda e: e.iota(pidx.ap, [[0, 1]], base=0, channel_multiplier=1,
                                      allow_small_or_imprecise_dtypes=True), writes=[pidx])
        pos0 = self.b_pos0
        P.op(P.pool, lambda e: e.iota(pos0.ap, [[1, 512]], base=0, channel_multiplier=0,
                                      allow_small_or_imprecise_dtypes=True), writes=[pos0])
        for h in range(4):
            lg = LOG_GAMMAS[h]
            self.act_fn(P.sub(self.b_DT, h * 128, (h + 1) * 128, F32), rel, AF.Exp, scale=lg)
            self.act_fn(P.sub(self.b_qdec, h * 128, (h + 1) * 128, F32), t1, AF.Exp, scale=lg)
            self.act_fn(P.sub(self.b_kdec, h, h + 1, F32), k1, AF.Exp, scale=lg)
        DT = self.b_DT
        P.op(P.pool, lambda e: e.affine_select(DT.ap, DT.ap, [[0, 4], [1, 128]], ALU.is_ge, 0.0, base=0,
                                               channel_multiplier=-1), reads=[DT], writes=[DT])
        self.act_fn(self.b_inv, pidx, AF.Exp, scale=-math.log(10000.0) / 128.0)
        for h in range(4):
            for half in range(2):
                S, Sb = self.S[h][half], self.Sbf[h][half]
                P.op(P.pool, lambda e, S=S: e.memset(S.ap, 0.0), writes=[S])
                P.op(P.pool, lambda e, Sb=Sb: e.memset(Sb.ap, 0.0), writes=[Sb])
        P.sp_off = mark

    def rotary_tables(self, ti, background=False):
        P = self.P
        PI = math.pi
        TWO_PI = 2.0 * math.pi
        C1 = 6.28125
        C2 = TWO_PI - C1
        cos_t, sin_t, cos16, sin16 = self.rot_tabs
        ang, tfix, rc = self.rot_tmp
        base = ti * self.T
        ops = []
        ops.append(lambda: self.ts(ang, self.b_pos0, float(base), self.b_inv.ap[:, 0:1], ALU.add, ALU.mult,
                                   extra_reads=[self.b_inv]))
        for b in (512, 256, 128, 64, 32, 16, 8, 4, 2, 1):
            ops.append(lambda b=b: self.ts(tfix, ang, float(b * TWO_PI), None, ALU.is_ge))
            ops.append(lambda b=b: self.stt(ang, tfix, -float(b * C1), ang, ALU.mult, ALU.add))
            ops.append(lambda b=b: self.stt(ang, tfix, -float(b * C2), ang, ALU.mult, ALU.add))
        ops.append(lambda: self.ts(ang, ang, -PI, None, ALU.add))
        ops.append(lambda: self.ts(tfix, ang, PI, TWO_PI, ALU.is_gt, ALU.mult))
        ops.append(lambda: self.tt(ang, ang, tfix, ALU.subtract))
        ops.append(lambda: self.ts(tfix, ang, -PI, TWO_PI, ALU.is_lt, ALU.mult))
        ops.append(lambda: self.tt(ang, ang, tfix, ALU.add))
        ops.append(lambda: self.ts(rc, ang, PI / 2.0, None, ALU.add))
        ops.append(lambda: self.ts(tfix, rc, PI, TWO_PI, ALU.is_gt, ALU.mult))
        ops.append(lambda: self.tt(rc, rc, tfix, ALU.subtract))
        ops.append(lambda: self.act_fn(sin_t, ang, AF.Sin, scale=-1.0))
        ops.append(lambda: self.act_fn(cos_t, rc, AF.Sin, scale=-1.0))
        ops.append(lambda: self.ts(sin16, sin_t, 1.0 / 16.0, None, ALU.mult))
        ops.append(lambda: self.ts(cos16, cos_t, 1.0 / 16.0, None, ALU.mult))
        if background:
            self.bg_ops.extend(ops)
        else:
            for f in ops:
                f()

    def bg_step(self, n=2):
        for _ in range(n):
            if self.bg_ops:
                self.bg_ops.pop(0)()

    def mixer_b(self, lnidx, ti):
        P = self.P
        NG = self.NG
        mark = P.sp_off
        w_in = self.b_w_in[0].rearrange("(k p) f -> p k f", p=128)
        w_out = self.b_w_out[0].rearrange("(f p) d -> p f d", p=128)
        PI = math.pi
        TWO_PI = 2.0 * math.pi
        C1 = 6.28125
        C2 = TWO_PI - C1
        pbanks = [0, 1, 2]

        def pbank():
            b = pbanks[self.mm_i % 3]
            self.mm_i += 1
            return b

        self.bg_step(1000)
        z_off = P.sp_off
        cos_t, sin_t, cos16, sin16 = self.rot_tabs
        ang, kf, tfix, rc = (P.tile(512, F32) for _ in range(4))
        ta, tb, tc, td = ang, kf, tfix, rc
        qT = [P.tile(512, BF16) for _ in range(2)]
        kT = [P.tile(512, BF16) for _ in range(2)]
        qd = [P.tile(512, BF16) for _ in range(2)]
        vh = [P.tile(512, BF16) for _ in range(4)]
        kd = [P.tile(256, BF16) for _ in range(4)]
        sc = [P.tile(128, BF16) for _ in range(2)]
        yh = P.tile(2048, F32)
        yb = [P.tile(512, BF16) for _ in range(4)]
        ysq = [P.tile(512, BF16) for _ in range(4)]
        gated = [[P.tile(512, BF16) for g in range(NG)] for f in range(16)]
        sgh = [P.tile(512, F32) for _ in range(4)]
        sci = 0
        for g in range(NG):
            def do_qk(h):
                qdec_b = self.b_qdec.ap[:, h * 128:(h + 1) * 128].unsqueeze(1).broadcast_to([128, 4, 128])
                wq = self.wload(w_in[:, :, h * 256:h * 256 + 256], [128, KD, 256])
                wk = self.wload(w_in[:, :, 1024 + h * 256:1024 + h * 256 + 256], [128, KD, 256])
                if h == 0 and NG == 1:
                    qkb = [P.bank(0), P.bank(1), P.bank(2), P.bank(4)]
                    self.kouter([(qkb[i],
                                  (lambda k, i=i: (wq if i < 2 else wk).ap[:, k, (i % 2) * 128:(i % 2 + 1) * 128]),
                                  (lambda k: self.xop[k][g].ap),
                                  (lambda k, i=i: [wq if i < 2 else wk, self.xop[k][g]])) for i in range(4)])
                else:
                    qkb = [P.bank(pbank()) for _ in range(3)] + [P.bank(self.next_y_bank())]
                    for i in range(4):
                        wb = wq if i < 2 else wk
                        for k in range(KD):
                            self.mm(qkb[i], wb.ap[:, k, (i % 2) * 128:(i % 2 + 1) * 128], self.xop[k][g].ap, k == 0,
                                    k == KD - 1, reads=[wb, self.xop[k][g]])
                for which in range(2):
                    b1, b2 = qkb[2 * which], qkb[2 * which + 1]
                    cs, sn = (cos_t, sin_t) if which == 0 else (cos16, sin16)
                    dst = qT if which == 0 else kT
                    self.tt(ta, b1, cs, ALU.mult)
                    self.tt(tc, b1, sn, ALU.mult)
                    self.tt(tb, b2, sn, ALU.mult)
                    self.tt(td, b2, cs, ALU.mult)
                    self.tt(dst[0], ta, tb, ALU.subtract)
                    self.tt(dst[1], tc, td, ALU.add)
                    if which == 0:
                        for half in range(2):
                            o3 = qd[half].ap.rearrange("p (c t) -> p c t", t=128)
                            i3 = qT[half].ap.rearrange("p (c t) -> p c t", t=128)
                            P.op(P.dve, lambda e, o3=o3, i3=i3, qdec_b=qdec_b: e.tensor_tensor(o3, i3, qdec_b, ALU.mult),
                                 reads=[qT[half], self.b_qdec], writes=[qd[half]])

            def do_vkt(h):
                wb = self.wload(w_in[:, :, 2048 + h * 512:2048 + (h + 1) * 512], [128, KD, 512])
                for c in range(4):
                    bk = P.bank(pbank())
                    for k in range(KD):
                        self.mm(bk, self.xop[k][g].ap[:, c * 128:(c + 1) * 128], wb.ap[:, k, :], k == 0, k == KD - 1,
                                reads=[wb, self.xop[k][g]])
                    self.act_fn(vh[c], bk, AF.Copy)
                for c in range(4):
                    b = pbank()
                    for half in range(2):
                        o = P.bank(b, half * 128, (half + 1) * 128)
                        self.mm(o, kT[half].ap[:, c * 128:(c + 1) * 128], self.ident_bf.ap, True, True,
                                reads=[kT[half], self.ident_bf], signal=(half == 1))
                    bk = P.bank(b, 0, 256)
                    self.act_fn(kd[c], bk, AF.Identity, scale=self.b_kdec.ap[:, h:h + 1], extra_reads=[self.b_kdec])

            def do_chunks(h):
                nonlocal sci
                wgb = self.wload(w_in[:, :, 4096 + h * 512:4096 + (h + 1) * 512], [128, KD, 512])
                yh3 = yh.ap.rearrange("p (v t) -> p v t", t=512)
                for c in range(4):
                    cs_ = slice(c * 128, (c + 1) * 128)
                    bs = P.bank(3, 0, 128)
                    for half in range(2):
                        self.mm(bs, kT[half].ap[:, cs_], qT[half].ap[:, cs_], half == 0, half == 1,
                                reads=[kT[half], qT[half]])
                    sct = sc[sci % 2]
                    sci += 1
                    dts = P.sub(self.b_DT, h * 128, (h + 1) * 128, F32)
                    self.tt(sct, bs, dts, ALU.mult)
                    bk = P.bank(pbank())
                    for k in range(KD):
                        self.mm(bk, wgb.ap[:, k, c * 128:(c + 1) * 128], self.xop[k][g].ap, k == 0, k == KD - 1,
                                reads=[wgb, self.xop[k][g]])
                    self.act_fn(sgh[c], bk, AF.Silu)
                    by_b = self.next_y_bank()
                    for vc in range(4):
                        o = P.bank(by_b, vc * 128, (vc + 1) * 128)
                        self.mm(o, vh[c].ap[:, vc * 128:(vc + 1) * 128], sct.ap, True, False, reads=[vh[c], sct], signal=False)
                        for half in range(2):
                            self.mm(o, self.Sbf[h][half].ap[:, vc * 128:(vc + 1) * 128], qd[half].ap[:, cs_], False, half == 1,
                                    reads=[self.Sbf[h][half], qd[half]], signal=(half == 1 and vc == 3))
                    by = P.bank(by_b)
                    o_ap = yh3[:, :, cs_]
                    i_ap = by.ap.rearrange("p (v t) -> p v t", t=128)
                    P.op(P.act, lambda e, o_ap=o_ap, i_ap=i_ap: e.activation(o_ap, i_ap, AF.Copy), reads=[by], writes=[yh])
                    for half in range(2):
                        bS = P.bank(6 + half)
                        self.mm(bS, kd[c].ap[:, half * 128:(half + 1) * 128], vh[c].ap, True, True, reads=[kd[c], vh[c]])
                        S, Sb = self.S[h][half], self.Sbf[h][half]
                        self.stt(S, S, GAMMAS[h] ** 128, bS, ALU.mult, ALU.add)
                        self.act_fn(Sb, S, AF.Copy)

            def do_gn(h):
                yv = [P.sub(yh, vc * 512, (vc + 1) * 512, F32) for vc in range(4)]
                for vc in range(4):
                    self.act_fn(yb[vc], yv[vc], AF.Copy)
                    self.act_fn(ysq[vc], yv[vc], AF.Square)
                s1, s2 = P.bank(6), P.bank(7)
                for vc in range(4):
                    self.mm(s1, self.ones_bf.ap, yb[vc].ap, vc == 0, vc == 3, reads=[self.ones_bf, yb[vc]])
                for vc in range(4):
                    self.mm(s2, self.ones_bf.ap, ysq[vc].ap, vc == 0, vc == 3, reads=[self.ones_bf, ysq[vc]])
                m, v, r = self.m_t, self.v_t, self.r_t
                self.ts(m, s1, 1.0 / 512, None, ALU.mult)
                self.tt(v, m, m, ALU.mult)
                self.stt(v, s2, 1.0 / 512, v, ALU.mult, ALU.subtract)
                self.act_fn(r, v, AF.Ln, bias=self.const_ap(GN_EPS), extra_reads=[self.cst])
                self.act_fn(r, r, AF.Exp, scale=-0.5)
                for vc in range(4):
                    self.tt(yv[vc], yv[vc], m, ALU.subtract)
                    self.tt(yv[vc], yv[vc], r, ALU.mult)
                    self.tt(gated[h * 4 + vc][g], yv[vc], sgh[vc], ALU.mult)

            do_qk(0)
            self.flush_pending()
            for h in range(4):
                do_vkt(h)
                do_chunks(h)
                if h < 3:
                    do_qk(h + 1)
                do_gn(h)
        wo_slots = {}

        def yfn(dc, g, bk):
            if g == 0 or dc not in wo_slots:
                wo_slots[dc] = self.wload(w_out[:, :, dc * 128:(dc + 1) * 128], [128, 16, 128])
            sd = wo_slots[dc]
            for f in range(16):
                self.mm(bk, sd.ap[:, f, :], gated[f][g].ap, f == 0, f == 15, reads=[sd, gated[f][g]])

        self.resid_ln(lnidx, ALPHA, LN_EPS, yfn, zoff=z_off)
        P.sp_off = mark


def _build(nsub=6, ntiles=None, T=512):
    k = Kern(nsub=nsub, ntiles=ntiles, T=T)
    return k


_CACHE = {}


def kernel(**inputs):
    x = np.ascontiguousarray(inputs["x"], dtype=np.float32)
    if "nc" not in _CACHE:
        k = Kern()
        _CACHE["nc"] = k.build()
    nc = _CACHE["nc"]
    names = ["ln_g", "ln_b", "ffn_w_gate", "ffn_w_up", "ffn_w_down", "a_w_in", "a_ln_g", "a_ln_b",
             "a_w_s", "a_b_s", "a_w_out", "b_w_in", "b_w_out"]
    shared = {n: np.ascontiguousarray(inputs[n], dtype=np.float32) for n in names}
    in_maps = []
    for b in range(NB):
        m = dict(shared)
        m["x"] = x[b]
        in_maps.append(m)
    res = run_bass_kernel_spmd(nc, in_maps, core_ids=list(range(NB)))
    return np.stack([r["out"] for r in res.results], axis=0)
```
